# Optimizing a Trainium2 kernel written in Bass

```python
import math
import jax, jax.numpy as jnp
from jax import lax
import numpy as np

D_MODEL = 1024
BATCH = 2
SEQ = 8192
DEPTH = 4

GRID_W = 64
CTX_LEN = 256
Q_BLOCK = 128
EPS = 1e-6
ROPE_THETA = 10000.0

A_HEADS = 6
A_KV_HEADS = 2
A_GROUP = A_HEADS // A_KV_HEADS
A_HEAD_DIM = 128
A_Q_WIDTH = A_HEADS * A_HEAD_DIM
A_KV_WIDTH = A_KV_HEADS * A_HEAD_DIM
B_GROUPS = 4
B_GROUP_DIM = 64
B_WIDTH = B_GROUPS * B_GROUP_DIM
EVEN_IN = A_Q_WIDTH + 2 * A_KV_WIDTH + B_WIDTH
EVEN_OUT = A_Q_WIDTH + B_WIDTH
EVEN_SPLIT = (A_Q_WIDTH, A_Q_WIDTH + A_KV_WIDTH, A_Q_WIDTH + 2 * A_KV_WIDTH)
C_HEADS = 8
C_QK_DIM = 64
C_V_DIM = 128
C_QK_WIDTH = C_HEADS * 2 * C_QK_DIM
C_V_WIDTH = C_HEADS * C_V_DIM
ODD_IN = 2 * C_QK_WIDTH + C_V_WIDTH
ODD_OUT = C_V_WIDTH
N_GROUPS = 4
EXPERTS_PER_GROUP = 4
N_EXPERTS = N_GROUPS * EXPERTS_PER_GROUP
TOP_K = 2
D_EXPERT = 512

kernel_name = "hybrid_gqa_fnet_diffattn_hmoe_dit"


def rms_norm(x, gain=None):
    xf = x.astype(jnp.float32)
    y = xf * lax.rsqrt(jnp.mean(xf * xf, axis=-1, keepdims=True) + EPS)
    if gain is not None:
        y = y * gain.astype(jnp.float32)
    return y.astype(x.dtype)


def axial_rope_tables(n_tokens, dim):
    rows = n_tokens // GRID_W
    row = jnp.broadcast_to(jnp.arange(rows, dtype=jnp.float32)[:, None], (rows, GRID_W)).reshape(-1)
    col = jnp.broadcast_to(jnp.arange(GRID_W, dtype=jnp.float32)[None, :], (rows, GRID_W)).reshape(-1)
    half = dim // 2
    inv_freq = ROPE_THETA ** (-jnp.arange(0, half, 2, dtype=jnp.float32) / half)
    ang_r = row[:, None] * inv_freq
    ang_c = col[:, None] * inv_freq
    return (jnp.cos(ang_r), jnp.sin(ang_r), jnp.cos(ang_c), jnp.sin(ang_c))


def _rotate(x, cos, sin):
    x1, x2 = jnp.split(x, 2, axis=-1)
    c = cos[:, None, :].astype(x.dtype)
    s = sin[:, None, :].astype(x.dtype)
    return jnp.concatenate([x1 * c - x2 * s, x1 * s + x2 * c], axis=-1)


def apply_axial_rope(x, tables):
    cr, sr, cc, sc = tables
    xr, xc = jnp.split(x, 2, axis=-1)
    return jnp.concatenate([_rotate(xr, cr, sr), _rotate(xc, cc, sc)], axis=-1)


def gqa_blocked(q, k, v):
    b, n = q.shape[:2]
    nb = n // Q_BLOCK
    qb = q.reshape(b, nb, Q_BLOCK, A_KV_HEADS, A_GROUP, A_HEAD_DIM).swapaxes(0, 1)
    scale = A_HEAD_DIM ** -0.5

    def block(qi):
        s = jnp.einsum('bqhgd,bkhd->bhgqk', qi, k).astype(jnp.float32) * scale
        p = jax.nn.softmax(s, axis=-1).astype(v.dtype)
        return jnp.einsum('bhgqk,bkhd->bqhgd', p, v)

    o = lax.map(block, qb)
    return o.swapaxes(0, 1).reshape(b, n, A_Q_WIDTH)


def diff_blocked(q, k, v, lam):
    b, n = q.shape[:2]
    nb = n // Q_BLOCK
    qb = q.reshape(b, nb, Q_BLOCK, C_HEADS, 2, C_QK_DIM).swapaxes(0, 1)
    scale = C_QK_DIM ** -0.5

    def block(qi):
        s = jnp.einsum('bqhmd,bkhmd->bmhqk', qi, k).astype(jnp.float32) * scale
        p = jax.nn.softmax(s, axis=-1)
        diff = (p[:, 0] - lam * p[:, 1]).astype(v.dtype)
        return jnp.einsum('bhqk,bkhd->bqhd', diff, v)

    o = lax.map(block, qb)
    return o.swapaxes(0, 1).reshape(b, n, C_HEADS, C_V_DIM)


def fourier_mix(f, gain):
    b, n, _ = f.shape
    g = rms_norm(f.reshape(b, n, B_GROUPS, B_GROUP_DIM), gain)
    spec = jnp.fft.fft2(g.astype(jnp.float32), axes=(1, 3), norm='ortho')
    return jnp.real(spec).astype(f.dtype).reshape(b, n, B_WIDTH)


def even_mixer(h_lat, h_ctx, w_in, w_out, q_gain, k_gain, f_gain, rope, need_ctx_out):
    def project(h):
        p = h @ w_in
        b, n, _ = p.shape
        q, k, v, f = jnp.split(p, EVEN_SPLIT, axis=-1)
        q = rms_norm(q.reshape(b, n, A_HEADS, A_HEAD_DIM), q_gain)
        k = rms_norm(k.reshape(b, n, A_KV_HEADS, A_HEAD_DIM), k_gain)
        v = v.reshape(b, n, A_KV_HEADS, A_HEAD_DIM)
        return q, k, v, f

    q_l, k_l, v_l, f_l = project(h_lat)
    q_c, k_c, v_c, f_c = project(h_ctx)
    q_l = apply_axial_rope(q_l, rope)
    k_l = apply_axial_rope(k_l, rope)
    k_all = jnp.concatenate([k_c, k_l], axis=1)
    v_all = jnp.concatenate([v_c, v_l], axis=1)
    y_lat = jnp.concatenate([gqa_blocked(q_l, k_all, v_all), fourier_mix(f_l, f_gain)], axis=-1) @ w_out
    if not need_ctx_out:
        return y_lat, None
    y_ctx = jnp.concatenate([gqa_blocked(q_c, k_c, v_c), fourier_mix(f_c, f_gain)], axis=-1) @ w_out
    return y_lat, y_ctx


def odd_mixer(h_lat, h_ctx, w_in, w_out, lq1, lk1, lq2, lk2, head_gain, lam_init, rope, need_ctx_out):
    f32 = jnp.float32
    lam = (jnp.exp(jnp.sum(lq1.astype(f32) * lk1.astype(f32)))
           - jnp.exp(jnp.sum(lq2.astype(f32) * lk2.astype(f32))) + lam_init)

    def project(h):
        p = h @ w_in
        b, n, _ = p.shape
        q, k, v = jnp.split(p, (C_QK_WIDTH, 2 * C_QK_WIDTH), axis=-1)
        return (q.reshape(b, n, 2 * C_HEADS, C_QK_DIM), k.reshape(b, n, 2 * C_HEADS, C_QK_DIM),
                v.reshape(b, n, C_HEADS, C_V_DIM))

    def to_maps(t):
        b, n = t.shape[:2]
        return t.reshape(b, n, C_HEADS, 2, C_QK_DIM)

    def finish(o):
        b, n = o.shape[:2]
        o = rms_norm(o, head_gain) * (1.0 - lam_init)
        return o.reshape(b, n, C_V_WIDTH) @ w_out

    q_l, k_l, v_l = project(h_lat)
    q_c, k_c, v_c = project(h_ctx)
    q_l = to_maps(apply_axial_rope(q_l, rope))
    k_l = to_maps(apply_axial_rope(k_l, rope))
    q_c, k_c = to_maps(q_c), to_maps(k_c)
    k_all = jnp.concatenate([k_c, k_l], axis=1)
    v_all = jnp.concatenate([v_c, v_l], axis=1)
    y_lat = finish(diff_blocked(q_l, k_all, v_all, lam))
    if not need_ctx_out:
        return y_lat, None
    return y_lat, finish(diff_blocked(q_c, k_c, v_c, lam))


def hierarchical_moe(h, w_group, b_group, w_router, b_router, w_gate, w_up, w_down):
    shape = h.shape
    tok = h.reshape(-1, shape[-1])
    g_logits = (tok @ w_group + b_group).astype(jnp.float32)
    g_prob = jax.nn.softmax(g_logits, axis=-1)
    g_idx = jnp.argmax(g_logits, axis=-1)
    g_gate = jnp.take_along_axis(g_prob, g_idx[:, None], axis=-1)
    e_logits = (tok @ w_router + b_router).astype(jnp.float32).reshape(-1, N_GROUPS, EXPERTS_PER_GROUP)
    e_sel = jnp.take_along_axis(e_logits, g_idx[:, None, None], axis=1)[:, 0]
    top_p, top_i = lax.top_k(jax.nn.softmax(e_sel, axis=-1), TOP_K)
    weights = g_gate * top_p / jnp.sum(top_p, axis=-1, keepdims=True)
    expert_id = g_idx[:, None] * EXPERTS_PER_GROUP + top_i
    dense_gate = jnp.einsum('nk,nke->en', weights, jax.nn.one_hot(expert_id, N_EXPERTS, dtype=jnp.float32))

    def expert_step(acc, params):
        wg, wu, wd, gate = params
        y = (jax.nn.silu(tok @ wg) * (tok @ wu)) @ wd
        return acc + gate[:, None].astype(y.dtype) * y, None

    out, _ = lax.scan(expert_step, jnp.zeros_like(tok), (w_gate, w_up, w_down, dense_gate))
    return out.reshape(shape)


def setup_inputs(seed: int = 0) -> dict:
    key = jax.random.key(seed)
    ks = jax.random.split(key, 26)
    n_even = (DEPTH + 1) // 2
    n_odd = DEPTH // 2

    def nrm(k, shape, s):
        return jax.random.normal(k, shape, jnp.float32) * s

    def gain(k, shape):
        return 1.0 + 0.02 * jax.random.normal(k, shape, jnp.float32)

    return {
        "x": nrm(ks[0], (BATCH, SEQ, D_MODEL), 1.0),
        "c": nrm(ks[1], (BATCH, D_MODEL), 1.0),
        "ctx": nrm(ks[2], (BATCH, CTX_LEN, D_MODEL), 1.0),
        "c_ctx": nrm(ks[3], (D_MODEL,), 1.0),
        "w_ada": nrm(ks[4], (DEPTH, D_MODEL, 6 * D_MODEL), 0.5 * D_MODEL ** -0.5),
        "b_ada": nrm(ks[5], (DEPTH, 6 * D_MODEL), 0.01),
        "w_in_even": nrm(ks[6], (n_even, D_MODEL, EVEN_IN), D_MODEL ** -0.5),
        "w_out_even": nrm(ks[7], (n_even, EVEN_OUT, D_MODEL), EVEN_OUT ** -0.5),
        "a_q_gain": gain(ks[8], (n_even, A_HEAD_DIM)),
        "a_k_gain": gain(ks[9], (n_even, A_HEAD_DIM)),
        "b_gain": gain(ks[10], (n_even, B_GROUPS, B_GROUP_DIM)),
        "w_in_odd": nrm(ks[11], (n_odd, D_MODEL, ODD_IN), D_MODEL ** -0.5),
        "w_out_odd": nrm(ks[12], (n_odd, ODD_OUT, D_MODEL), ODD_OUT ** -0.5),
        "c_lambda_q1": nrm(ks[13], (n_odd, C_QK_DIM), 0.1),
        "c_lambda_k1": nrm(ks[14], (n_odd, C_QK_DIM), 0.1),
        "c_lambda_q2": nrm(ks[15], (n_odd, C_QK_DIM), 0.1),
        "c_lambda_k2": nrm(ks[16], (n_odd, C_QK_DIM), 0.1),
        "c_head_gain": gain(ks[17], (n_odd, C_V_DIM)),
        "w_group": nrm(ks[18], (DEPTH, D_MODEL, N_GROUPS), D_MODEL ** -0.5),
        "b_group": nrm(ks[19], (DEPTH, N_GROUPS), 0.01),
        "w_router": nrm(ks[20], (DEPTH, D_MODEL, N_EXPERTS), D_MODEL ** -0.5),
        "b_router": nrm(ks[21], (DEPTH, N_EXPERTS), 0.01),
        "w_gate": nrm(ks[22], (DEPTH, N_EXPERTS, D_MODEL, D_EXPERT), D_MODEL ** -0.5),
        "w_up": nrm(ks[23], (DEPTH, N_EXPERTS, D_MODEL, D_EXPERT), D_MODEL ** -0.5),
        "w_down": nrm(ks[24], (DEPTH, N_EXPERTS, D_EXPERT, D_MODEL), D_EXPERT ** -0.5),
        "final_gain": gain(ks[25], (D_MODEL,)),
    }


def reference(x, c, ctx, c_ctx, w_ada, b_ada, w_in_even, w_out_even, a_q_gain, a_k_gain, b_gain,
              w_in_odd, w_out_odd, c_lambda_q1, c_lambda_k1, c_lambda_q2, c_lambda_k2, c_head_gain,
              w_group, b_group, w_router, b_router, w_gate, w_up, w_down, final_gain):
    n_lat = x.shape[1]
    rope_a = axial_rope_tables(n_lat, A_HEAD_DIM)
    rope_c = axial_rope_tables(n_lat, C_QK_DIM)
    silu_c = jax.nn.silu(c)
    silu_cc = jax.nn.silu(c_ctx)
    for l in range(DEPTH):
        last = l == DEPTH - 1
        i = l // 2
        mod = silu_c @ w_ada[l] + b_ada[l]
        mod_c = silu_cc @ w_ada[l] + b_ada[l]
        sh1, sc1, g1, sh2, sc2, g2 = jnp.split(mod[:, None, :], 6, axis=-1)
        csh1, csc1, cg1, csh2, csc2, cg2 = jnp.split(mod_c, 6, axis=-1)
        h_lat = rms_norm(x) * (1 + sc1) + sh1
        h_ctx = rms_norm(ctx) * (1 + csc1) + csh1
        if l % 2 == 0:
            y_lat, y_ctx = even_mixer(h_lat, h_ctx, w_in_even[i], w_out_even[i], a_q_gain[i], a_k_gain[i],
                                      b_gain[i], rope_a, not last)
        else:
            lam_init = 0.8 - 0.6 * math.exp(-0.3 * l)
            y_lat, y_ctx = odd_mixer(h_lat, h_ctx, w_in_odd[i], w_out_odd[i], c_lambda_q1[i], c_lambda_k1[i],
                                     c_lambda_q2[i], c_lambda_k2[i], c_head_gain[i], lam_init, rope_c, not last)
        x = x + g1 * y_lat
        x = x + g2 * hierarchical_moe(rms_norm(x) * (1 + sc2) + sh2, w_group[l], b_group[l], w_router[l],
                                      b_router[l], w_gate[l], w_up[l], w_down[l])
        if not last:
            ctx = ctx + cg1 * y_ctx
            ctx = ctx + cg2 * hierarchical_moe(rms_norm(ctx) * (1 + csc2) + csh2, w_group[l], b_group[l],
                                               w_router[l], b_router[l], w_gate[l], w_up[l], w_down[l])
    return rms_norm(x, final_gain)
```

```python
import contextlib
import math
import numpy as np
import ml_dtypes
import concourse.bass as bass
import concourse.mybir as mybir
from concourse.bass_utils import run_bass_kernel_spmd

F32 = mybir.dt.float32
BF16 = mybir.dt.bfloat16
AF = mybir.ActivationFunctionType
ALU = mybir.AluOpType
AX = mybir.AxisListType
NPBF = ml_dtypes.bfloat16

NCORES = 8
D = 1024
KC = 8
SEQ = 8192
CTX = 256
TL = 2048
T = CTX + TL
NT = T // 128
CHUNKS = [(0, 256), (256, 512), (768, 512), (1280, 512), (1792, 512)]
NKEY = CTX + SEQ
NKT = NKEY // 128
EPS = 1e-6
DEPTH = 4


class Buf:
    __slots__ = ("name", "last_w", "readers")

    def __init__(self, name=""):
        self.name = name
        self.last_w = None
        self.readers = []


class _Rec:
    def __init__(self):
        self.call = None

    def __getattr__(self, name):
        def f(*a, **kw):
            self.call = (name, a, kw)
        return f


class Op:
    __slots__ = ("eng", "fn", "deps", "is_dma", "key", "cnt", "need_inc", "dma_val")

    def __init__(self, eng, fn, is_dma=False, key=None):
        self.eng = eng
        r = _Rec()
        fn(r)
        name, a, kw = r.call
        self.fn = lambda e: getattr(e, name)(*a, **kw)
        self.deps = []
        self.is_dma = is_dma
        self.key = key
        self.cnt = None
        self.need_inc = False
        self.dma_val = None


class Sched:
    ENGS = ("sync", "act", "dve", "pool", "pe")

    def __init__(self, nc):
        self.nc = nc
        self.q = {e: [] for e in self.ENGS}
        self.slot_of = {}
        self.slot_cnt = []
        self.slot_last = []
        self.out_dmas = []

    def fence(self):
        tails = [self.q[e][-1] for e in self.ENGS if self.q[e] and not self.q[e][-1].is_dma]
        for e in self.ENGS:
            for o in reversed(self.q[e]):
                if not o.is_dma:
                    if o not in tails:
                        tails.append(o)
                    break
        tails += [o for o in self.slot_last if o is not None]
        for e in self.ENGS:
            o = Op(e, lambda x: x.nop())
            for d in tails:
                if d.is_dma or d.eng != e:
                    o.deps.append(d)
                    if not d.is_dma:
                        d.need_inc = True
            self.q[e].append(o)
        self.slot_of = {}

    def _add(self, op, reads, writes):
        deps = []
        for b in reads:
            if b.last_w is not None:
                deps.append(("raw", b.last_w))
        for b in writes:
            if b.last_w is not None:
                deps.append(("waw", b.last_w))
            for r in b.readers:
                deps.append(("war", r))
        for kind, d in deps:
            if d is op:
                continue
            if d.is_dma:
                op.deps.append(d)
            elif d.eng == op.eng and not op.is_dma:
                if kind == "raw" and op.eng != "pe":
                    op.deps.append(d)
                    d.need_inc = True
            else:
                op.deps.append(d)
                d.need_inc = True
        for b in reads:
            b.readers.append(op)
        for b in writes:
            b.last_w = op
            b.readers = []
        self.q[op.eng].append(op)
        return op

    def op(self, eng, fn, reads=(), writes=()):
        return self._add(Op(eng, fn), list(reads), list(writes))

    def dma(self, eng, fn, key, reads=(), writes=(), is_out=False):
        if key not in self.slot_of:
            self.slot_of[key] = len(self.slot_of)
            if len(self.slot_cnt) < len(self.slot_of):
                self.slot_cnt.append(0)
                self.slot_last.append(None)
        slot = self.slot_of[key]
        o = Op(eng, fn, is_dma=True, key=slot)
        self.slot_cnt[slot] += 1
        o.dma_val = 16 * self.slot_cnt[slot]
        self.slot_last[slot] = o
        self._add(o, list(reads), list(writes))
        if is_out:
            self.out_dmas.append(o)
        return o

    def emit(self):
        nc = self.nc
        with contextlib.ExitStack() as st:
            esem = {e: st.enter_context(nc.semaphore("s_" + e)) for e in self.ENGS}
            dsem = {i: st.enter_context(nc.semaphore("d_%d" % i)) for i in range(len(self.slot_cnt))}
            for e in self.ENGS:
                c = 0
                for o in self.q[e]:
                    if (not o.is_dma) and o.need_inc:
                        c += 1
                        o.cnt = c
            block = st.enter_context(nc.Block())
            finals = {}
            for d in self.out_dmas:
                finals[d.key] = max(finals.get(d.key, 0), d.dma_val)

            def run(ename, eng):
                waited = {}
                for o in self.q[ename]:
                    for d in o.deps:
                        if d.is_dma:
                            s, v = dsem[d.key], d.dma_val
                        else:
                            s, v = esem[d.eng], d.cnt
                        kk = id(s)
                        if waited.get(kk, 0) >= v:
                            continue
                        waited[kk] = v
                        eng.wait_ge(s, v)
                    ins = o.fn(eng)
                    if o.is_dma:
                        ins.then_inc(dsem[o.key], 16)
                    elif o.need_inc:
                        ins.then_inc(esem[ename], 1)
                if ename == "sync":
                    for k, v in finals.items():
                        eng.wait_ge(dsem[k], v)

            @block.sync
            def _(e):
                run("sync", e)

            @block.scalar
            def _(e):
                run("act", e)

            @block.vector
            def _(e):
                run("dve", e)

            @block.gpsimd
            def _(e):
                run("pool", e)

            @block.tensor
            def _(e):
                run("pe", e)


ARENA_LO = 16512
ARENA_HI = 229376


class Prog:
    def __init__(self):
        self.nc = bass.Bass("TRN2", target_bir_lowering=False)
        self.S = Sched(self.nc)
        self.st = contextlib.ExitStack()
        self.top = ARENA_LO
        self.uid = 0

    def din(self, name, shape, dt):
        return self.nc.dram_tensor(name, list(shape), dt, kind="ExternalInput").ap()

    def dout(self, name, shape, dt):
        return self.nc.dram_tensor(name, list(shape), dt, kind="ExternalOutput").ap()

    def dscr(self, name, shape, dt):
        return self.nc.dram_tensor(name, list(shape), dt).ap()

    def sb(self, name, shape, dt):
        self.uid += 1
        esz = 2 if dt == BF16 else 4
        nbytes = esz
        for d in shape[1:]:
            nbytes *= d
        nbytes = (nbytes + 63) // 64 * 64
        assert self.top + nbytes <= ARENA_HI, ("SBUF arena overflow", name, self.top, nbytes)
        t = self.nc.alloc_sbuf_tensor_at("s%d_%s" % (self.uid, name), list(shape), dt, offset=self.top)
        self.top += nbytes
        return t, Buf(name)

    def mark(self):
        return self.top

    def release(self, mark):
        self.S.fence()
        self.top = mark

    def ps(self, name, shape=(128, 512), dt=F32):
        t = self.st.enter_context(self.nc.psum_tensor("p_" + name, list(shape), dt))
        return t, Buf(name)

    def ring(self, name, n, shape, dt):
        return Ring([self.sb("%s%d" % (name, i), shape, dt) for i in range(n)])

    def finish(self):
        self.S.emit()
        self.st.close()
        return self.nc


class Ring:
    def __init__(self, items):
        self.items = items
        self.i = 0

    def next(self):
        it = self.items[self.i % len(self.items)]
        self.i += 1
        return it


def emit_rstd(P, ss_ps, b_ss, n, inv_count, rs, b_rs):
    S = P.S
    S.op("act", lambda e: e.activation(out=rs[:, :n], in_=ss_ps[:, :n], func=AF.Sqrt, bias=P.epsc[:, 0:1], scale=inv_count),
         reads=[b_ss, P.b_eps], writes=[b_rs])
    S.op("dve", lambda e: e.reciprocal(out=rs[:, :n], in_=rs[:, :n]), reads=[b_rs], writes=[b_rs])


class XSrc:
    def __init__(self, P, xT_d, resident):
        self.P, self.xT_d, self.resident = P, xT_d, resident
        self.got = {}
        if resident:
            self.xT, _ = P.sb("xT", [128, KC, T], F32)
            self.bufs = [Buf("x%d" % i) for i in range(len(CHUNKS))]
        else:
            self.ring = P.ring("xr", 2, [128, KC, 512], F32)

    def get(self, ci):
        if ci in self.got:
            return self.got[ci]
        t0, n = CHUNKS[ci]
        if self.resident:
            tile, c0, buf = self.xT, t0, self.bufs[ci]
        else:
            tile, buf = self.ring.next()
            c0 = 0
        self.P.S.dma("sync", lambda e: e.dma_start(out=tile[:, :, c0:c0 + n], in_=self.xT_d[:, :, t0:t0 + n]), key=buf, writes=[buf])
        self.got[ci] = (tile, c0, buf)
        return self.got[ci]


def emit_norm_mod(P, xs, hT, b_h, j_sh, j_sc, ps_ring, sq_ring, rs_ring, tmp_ring, skip0=False):
    S = P.S
    modT, b_mod, scp = P.modT, P.b_mod, P.scp
    xs.get(1 if skip0 else 0)
    for ci, (t0, n) in enumerate(CHUNKS):
        if skip0 and ci == 0:
            continue
        t = 0 if ci == 0 else 1
        if ci + 1 < len(CHUNKS):
            xs.get(ci + 1)
        xt, c0, b_xc = xs.get(ci)
        ss, b_ss = ps_ring.next()
        for k in range(KC):
            sq, b_sq = sq_ring.next()
            S.op("act", lambda e: e.activation(out=sq[:, :n], in_=xt[:, k, c0:c0 + n], func=AF.Square), reads=[b_xc], writes=[b_sq])
            S.op("pe", lambda e: e.matmul(ss[:, :n], lhsT=P.ones[:], rhs=sq[:, :n], start=(k == 0), stop=(k == KC - 1)),
                 reads=[b_sq, P.b_ones], writes=[b_ss])
        rs, b_rs = rs_ring.next()
        emit_rstd(P, ss, b_ss, n, 1.0 / D, rs, b_rs)
        for k in range(KC):
            tmp, b_tmp = tmp_ring.next()
            S.op("dve", lambda e: e.scalar_tensor_tensor(out=tmp[:, :n], in0=xt[:, k, c0:c0 + n], scalar=scp[:, j_sc + k, t:t + 1], in1=rs[:, :n],
                                                         op0=ALU.mult, op1=ALU.mult), reads=[b_xc, b_rs, b_mod], writes=[b_tmp])
            S.op("act", lambda e: e.activation(out=hT[:, k, t0:t0 + n], in_=tmp[:, :n], func=AF.Identity, bias=modT[:, j_sh + k, t:t + 1], scale=1.0),
                 reads=[b_tmp, b_mod], writes=[b_h[ci]])


def load_w_bf16(P, name, w_d, ncols, col_split=1):
    S = P.S
    w, b_w = P.sb(name, [128, KC, ncols], BF16)
    wv = w_d.rearrange("(k p) n -> p k n", p=128)
    cs = ncols // col_split
    for k in range(KC):
        for c in range(col_split):
            S.dma("pool", lambda e: e.dma_start(out=w[:, k, c * cs:(c + 1) * cs], in_=wv[:, k, c * cs:(c + 1) * cs]), key=b_w, writes=[b_w])
    return w, b_w


def phase_mod(P, l):
    S = P.S
    mk = P.mark()
    D_ = P.D
    nj = 48
    wring = P.ring("wada", 2, [128, KC, 512], BF16)
    bT, b_bT = P.sb("bT", [128, nj], F32)
    S.dma("sync", lambda e: e.dma_start(out=bT[:], in_=D_["b_ada"][l]), key=b_bT, writes=[b_bT])
    pm, b_pm = P.bank[7]
    wv = D_["w_ada"][l].rearrange("(k p) n -> p k n", p=128)
    for blk in range(12):
        wt, b_wt = wring.next()
        for k in range(KC):
            S.dma("pool", lambda e: e.dma_start(out=wt[:, k, :], in_=wv[:, k, blk * 512:(blk + 1) * 512]), key=b_wt, writes=[b_wt])
        for jj in range(4):
            j = blk * 4 + jj
            for k in range(KC):
                S.op("pe", lambda e: e.matmul(pm[:, 2 * j:2 * j + 2], lhsT=wt[:, k, jj * 128:(jj + 1) * 128], rhs=P.cvb[:, k, :],
                                              start=(k == 0), stop=(k == KC - 1)), reads=[b_wt, P.b_cvb], writes=[b_pm])
    pmv = pm[:, 0:2 * nj].rearrange("p (j t) -> p j t", t=2)
    for t in range(2):
        S.op("dve", lambda e: e.tensor_tensor(out=P.modT[:, :, t], in0=pmv[:, :, t], in1=bT[:], op=ALU.add), reads=[b_pm, b_bT], writes=[P.b_mod])
    S.op("dve", lambda e: e.tensor_scalar(out=P.scp[:], in0=P.modT[:], scalar1=1.0, scalar2=None, op0=ALU.add), reads=[P.b_mod], writes=[P.b_mod])
    P.release(mk)


def phase_A(P, l, q, Xin):
    S = P.S
    D_ = P.D
    mk = P.mark()
    even = (l % 2 == 0)
    i = l // 2
    nq = 6 if even else 8
    nk = 2 if even else 8
    vw = 256 if even else 1024
    win_cols = 1536 if even else 3072
    win_d = (D_["w_in_even"] if even else D_["w_in_odd"])[i]
    cosT_d = (D_["ropecos128"] if even else D_["ropecos64"])[q]
    sinT_d = (D_["ropesin128"] if even else D_["ropesin64"])[q]
    PT = P.PT128 if even else P.PT64
    b_PT = P.b_const
    psA = Ring([P.bank[0], P.bank[1], P.bank[2]])
    psB = Ring([P.bank[3], P.bank[4]])

    xs = XSrc(P, Xin[q], resident=False)
    hT, _ = P.sb("hT", [128, KC, T], BF16)
    b_h = [Buf("h%d" % c) for c in range(len(CHUNKS))]
    sq_ring = P.ring("sq", 2, [128, 512], BF16)
    rs_ring = P.ring("rs", 2, [128, 512], F32)
    tmp_ring = P.ring("tmp", 2, [128, 512], F32)
    skip0 = (q != 0)
    emit_norm_mod(P, xs, hT, b_h, 0, KC, psB, sq_ring, rs_ring, tmp_ring, skip0=skip0)

    w, b_w = load_w_bf16(P, "win", win_d, win_cols, col_split=(1 if even else 2))
    cosT, b_cos = P.sb("cosT", [128, T], F32)
    sinT, b_sin = P.sb("sinT", [128, T], F32)
    S.dma("sync", lambda e: e.dma_start(out=cosT[:], in_=cosT_d), key=b_cos, writes=[b_cos])
    S.dma("sync", lambda e: e.dma_start(out=sinT[:], in_=sinT_d), key=b_sin, writes=[b_sin])
    if even:
        gains, b_g = P.sb("gains", [128, 4], F32)
        S.dma("sync", lambda e: e.dma_start(out=gains[:], in_=D_["gains"][i]), key=b_g, writes=[b_g])

    qn_ring = P.ring("qn", 2, [128, 512], F32)
    qnb_ring = P.ring("qnb", 2, [128, 512], BF16)
    t1_ring = P.ring("t1", 2, [128, 512], F32)
    t2_ring = P.ring("t2", 2, [128, 512], F32)
    stage_ring = P.ring("stg", 2, [128, T], BF16)
    Qs, Ks = P.Qs, P.Ks

    for hc in range(nq + nk):
        is_q = hc < nq
        c0 = hc * 128
        stg, b_stg = stage_ring.next()
        for ci, (t0, n) in enumerate(CHUNKS):
            if skip0 and ci == 0:
                continue
            pq, b_pq = psA.next()
            for k in range(KC):
                S.op("pe", lambda e: e.matmul(pq[:, :n], lhsT=w[:, k, c0:c0 + 128], rhs=hT[:, k, t0:t0 + n], start=(k == 0), stop=(k == KC - 1)),
                     reads=[b_w, b_h[ci]], writes=[b_pq])
            qn, b_qn = qn_ring.next()
            if even:
                sq, b_sq = sq_ring.next()
                S.op("act", lambda e: e.activation(out=sq[:, :n], in_=pq[:, :n], func=AF.Square), reads=[b_pq], writes=[b_sq])
                ss, b_ss = psB.next()
                S.op("pe", lambda e: e.matmul(ss[:, :n], lhsT=P.ones[:], rhs=sq[:, :n], start=True, stop=True), reads=[b_sq, P.b_ones], writes=[b_ss])
                rs, b_rs = rs_ring.next()
                emit_rstd(P, ss, b_ss, n, 1.0 / 128, rs, b_rs)
                gcol = 0 if is_q else 1
                S.op("dve", lambda e: e.scalar_tensor_tensor(out=qn[:, :n], in0=pq[:, :n], scalar=gains[:, gcol:gcol + 1], in1=rs[:, :n],
                                                             op0=ALU.mult, op1=ALU.mult), reads=[b_pq, b_rs, b_g], writes=[b_qn])
            else:
                S.op("act", lambda e: e.copy(out=qn[:, :n], in_=pq[:, :n]), reads=[b_pq], writes=[b_qn])
            qnb, b_qnb = qnb_ring.next()
            S.op("act", lambda e: e.copy(out=qnb[:, :n], in_=qn[:, :n]), reads=[b_qn], writes=[b_qnb])
            pr, b_pr = psB.next()
            S.op("pe", lambda e: e.matmul(pr[:, :n], lhsT=PT[:], rhs=qnb[:, :n], start=True, stop=True), reads=[b_qnb, b_PT], writes=[b_pr])
            t1, b_t1 = t1_ring.next()
            S.op("pool", lambda e: e.tensor_tensor(out=t1[:, :n], in0=qn[:, :n], in1=cosT[:, t0:t0 + n], op=ALU.mult), reads=[b_qn, b_cos], writes=[b_t1])
            t2, b_t2 = t2_ring.next()
            S.op("dve", lambda e: e.tensor_tensor(out=t2[:, :n], in0=pr[:, :n], in1=sinT[:, t0:t0 + n], op=ALU.mult), reads=[b_pr, b_sin], writes=[b_t2])
            S.op("dve", lambda e: e.tensor_tensor(out=stg[:, t0:t0 + n], in0=t1[:, :n], in1=t2[:, :n], op=ALU.add), reads=[b_t1, b_t2], writes=[b_stg])
        if is_q:
            c_lo = CTX if skip0 else 0
            S.dma("sync", lambda e: e.dma_start(out=Qs[q, hc, :, c_lo:T], in_=stg[:, c_lo:T]), key=b_stg, reads=[b_stg])
        else:
            kh = hc - nq
            if q == 0:
                S.dma("sync", lambda e: e.dma_start(out=Ks[kh, :, 0:CTX], in_=stg[:, 0:CTX]), key=b_stg, reads=[b_stg])
            S.dma("sync", lambda e: e.dma_start(out=Ks[kh, :, CTX + q * TL:CTX + (q + 1) * TL], in_=stg[:, CTX:T]), key=b_stg, reads=[b_stg])

    vc0 = (nq + nk) * 128
    vst_ring = P.ring("vst", 2, [128, vw], BF16)
    for tt in range(NT):
        if tt < 2 and q != 0:
            continue
        gt = tt if tt < 2 else 2 + q * 16 + (tt - 2)
        ci = 0 if tt < 2 else 1 + (tt - 2) // 4
        vst, b_vst = vst_ring.next()
        cw = min(vw, 512)
        for cb in range(max(1, vw // 512)):
            pv, b_pv = psA.next()
            for k in range(KC):
                S.op("pe", lambda e: e.matmul(pv[:, :cw], lhsT=hT[:, k, tt * 128:(tt + 1) * 128], rhs=w[:, k, vc0 + cb * cw:vc0 + (cb + 1) * cw],
                                              start=(k == 0), stop=(k == KC - 1)), reads=[b_w, b_h[ci]], writes=[b_pv])
            S.op("act", lambda e: e.copy(out=vst[:, cb * cw:(cb + 1) * cw], in_=pv[:, :cw]), reads=[b_pv], writes=[b_vst])
        if even:
            S.dma("sync", lambda e: e.dma_start(out=P.Vs[:, gt, :], in_=vst[:]), key=b_vst, reads=[b_vst])
        else:
            S.dma("sync", lambda e: e.dma_start(out=P.Vo[:, :, gt, :].rearrange("h p d -> p h d"), in_=vst[:].rearrange("p (h d) -> p h d", h=8)),
                  key=b_vst, reads=[b_vst])

    if even:
        fc0 = 1280
        gcst, b_gcst = P.sb("gcst", [128, NT, 256], BF16)
        gsst, b_gsst = P.sb("gsst", [128, NT, 256], BF16)
        gT_ring = P.ring("gT", 2, [128, 512], BF16)
        for jc in range(2):
            for ci, (t0, n) in enumerate(CHUNKS):
                if ci == 0 and q != 0:
                    continue
                pf, b_pf = psA.next()
                for k in range(KC):
                    S.op("pe", lambda e: e.matmul(pf[:, :n], lhsT=w[:, k, fc0 + jc * 128:fc0 + (jc + 1) * 128], rhs=hT[:, k, t0:t0 + n],
                                                  start=(k == 0), stop=(k == KC - 1)), reads=[b_w, b_h[ci]], writes=[b_pf])
                sq, b_sq = sq_ring.next()
                S.op("act", lambda e: e.activation(out=sq[:, :n], in_=pf[:, :n], func=AF.Square), reads=[b_pf], writes=[b_sq])
                ss, b_ss = psB.next()
                S.op("pe", lambda e: e.matmul(ss[:, :n], lhsT=P.bdo[:], rhs=sq[:, :n], start=True, stop=True), reads=[b_sq, P.b_const], writes=[b_ss])
                rs, b_rs = rs_ring.next()
                emit_rstd(P, ss, b_ss, n, 1.0 / 64, rs, b_rs)
                gT, b_gT = gT_ring.next()
                S.op("dve", lambda e: e.scalar_tensor_tensor(out=gT[:, :n], in0=pf[:, :n], scalar=gains[:, 2 + jc:3 + jc], in1=rs[:, :n],
                                                             op0=ALU.mult, op1=ALU.mult), reads=[b_pf, b_rs, b_g], writes=[b_gT])
                for ti in range(n // 128):
                    tt = t0 // 128 + ti
                    pg, b_pg = psB.next()
                    S.op("pe", lambda e: e.matmul(pg[:, 0:128], lhsT=gT[:, ti * 128:(ti + 1) * 128], rhs=P.bdc[:], start=True, stop=True),
                         reads=[b_gT, P.b_const], writes=[b_pg])
                    S.op("pe", lambda e: e.matmul(pg[:, 128:256], lhsT=gT[:, ti * 128:(ti + 1) * 128], rhs=P.bds[:], start=True, stop=True),
                         reads=[b_gT, P.b_const], writes=[b_pg])
                    S.op("act", lambda e: e.copy(out=gcst[:, tt, jc * 128:(jc + 1) * 128], in_=pg[:, 0:128]), reads=[b_pg], writes=[b_gcst])
                    S.op("act", lambda e: e.copy(out=gsst[:, tt, jc * 128:(jc + 1) * 128], in_=pg[:, 128:256]), reads=[b_pg], writes=[b_gsst])
        for st_, sc_, cc_, bb in ((gcst, P.GCs, P.GCc, b_gcst), (gsst, P.GSs, P.GSc, b_gsst)):
            if q == 0:
                S.dma("sync", lambda e: e.dma_start(out=cc_, in_=st_[:, 0:2, :]), key=bb, reads=[bb])
            S.dma("sync", lambda e: e.dma_start(out=sc_[:, q * 16:(q + 1) * 16, :], in_=st_[:, 2:NT, :]), key=bb, reads=[bb])
    P.release(mk)


class AttnRes:
    pass


def attn_setup(P):
    R = AttnRes()
    R.S_ring = Ring([P.bank[0], P.bank[1], P.bank[2], P.bank[7]])
    R.O_ring = Ring([P.bank[3], P.bank[4]])
    R.Z_ring = Ring([P.bank[5], P.bank[6]])
    R.pt_ring = P.ring("pt", 6, [128, 512], BF16)
    R.rz_ring = P.ring("rz", 2, [128, 512], F32)
    R.accP_ring = P.ring("accP", 2, [128, 512], F32)
    R.accD_ring = P.ring("accD", 2, [128, 512], F32)
    return R


def attn_unit(P, R, q_ap, b_q, ktiles, vtiles, n, scale, bias_ap, out_ap, b_out):
    S = P.S
    po, b_po = R.O_ring.next()
    pz, b_pz = R.Z_ring.next()
    accs = [R.accP_ring.next(), R.accD_ring.next()]
    acc_eng = ["dve", "dve"]
    nk = len(ktiles)
    assert nk >= 2
    LA = 3
    pts = {}
    for i in range(nk + LA):
        if i < nk:
            kap, b_k = ktiles[i]
            ps, b_ps = R.S_ring.next()
            S.op("pe", lambda e: e.matmul(ps[:, :n], lhsT=kap, rhs=q_ap, start=True, stop=True), reads=[b_k, b_q], writes=[b_ps])
            pt, b_pt = R.pt_ring.next()
            S.op("act", lambda e: e.activation(out=pt[:, :n], in_=ps[:, :n], func=AF.Exp, bias=bias_ap, scale=scale), reads=[b_ps, P.b_const], writes=[b_pt])
            pts[i] = (pt, b_pt)
        j = i - LA
        if j >= 0:
            vap, b_v = vtiles[j]
            pt, b_pt = pts.pop(j)
            S.op("pe", lambda e: e.matmul(po[:, :n], lhsT=vap, rhs=pt[:, :n], start=(j == 0), stop=(j == nk - 1)), reads=[b_v, b_pt], writes=[b_po])
            acc, b_acc = accs[j % 2]
            if j < 2:
                S.op(acc_eng[j % 2], lambda e: e.tensor_copy(out=acc[:, :n], in_=pt[:, :n]), reads=[b_pt], writes=[b_acc])
            else:
                S.op(acc_eng[j % 2], lambda e: e.tensor_tensor(out=acc[:, :n], in0=acc[:, :n], in1=pt[:, :n], op=ALU.add), reads=[b_pt, b_acc], writes=[b_acc])
    S.op("dve", lambda e: e.tensor_tensor(out=accs[1][0][:, :n], in0=accs[1][0][:, :n], in1=accs[0][0][:, :n], op=ALU.add),
         reads=[accs[0][1], accs[1][1]], writes=[accs[1][1]])
    S.op("pe", lambda e: e.matmul(pz[:, :n], lhsT=P.ones32[:], rhs=accs[1][0][:, :n], start=True, stop=True), reads=[P.b_ones, accs[1][1]], writes=[b_pz])
    rz, b_rz = R.rz_ring.next()
    S.op("dve", lambda e: e.reciprocal(out=rz[:, :n], in_=pz[:, :n]), reads=[b_pz], writes=[b_rz])
    S.op("dve", lambda e: e.tensor_tensor(out=out_ap, in0=po[:, :n], in1=rz[:, :n], op=ALU.mult), reads=[b_po, b_rz], writes=[b_out])


def emit_outproj(P, mixT, b_mix, wout_d, xin_d, xout_d, j_g, ps_ring, skip0=False):
    S = P.S
    wo, b_wo = load_w_bf16(P, "wout", wout_d, D)
    xin_ring = P.ring("xin", 3, [128, 512], F32)
    xout_ring = P.ring("xout", 3, [128, 512], F32)
    for ci, (t0, n) in enumerate(CHUNKS):
        if skip0 and ci == 0:
            continue
        t = 0 if ci == 0 else 1
        for m in range(KC):
            py, b_py = ps_ring.next()
            for j in range(KC):
                S.op("pe", lambda e: e.matmul(py[:, :n], lhsT=wo[:, j, m * 128:(m + 1) * 128], rhs=mixT[:, j, t0:t0 + n], start=(j == 0), stop=(j == KC - 1)),
                     reads=[b_wo, b_mix[ci]], writes=[b_py])
            xi, b_xi = xin_ring.next()
            S.dma("sync", lambda e: e.dma_start(out=xi[:, :n], in_=xin_d[:, m, t0:t0 + n]), key=b_xi, writes=[b_xi])
            xo, b_xo = xout_ring.next()
            S.op("dve", lambda e: e.scalar_tensor_tensor(out=xo[:, :n], in0=py[:, :n], scalar=P.modT[:, j_g + m, t:t + 1], in1=xi[:, :n],
                                                         op0=ALU.mult, op1=ALU.add), reads=[b_py, b_xi, P.b_mod], writes=[b_xo])
            S.dma("sync", lambda e: e.dma_start(out=xout_d[:, m, t0:t0 + n], in_=xo[:, :n]), key=b_xo, reads=[b_xo])


def phase_B_even(P, l, q, Xin, Xout):
    S = P.S
    D_ = P.D
    mk = P.mark()
    i = l // 2
    R = attn_setup(P)
    mixT, _ = P.sb("mixT", [128, KC, T], BF16)
    b_mix = [Buf("mix%d" % c) for c in range(len(CHUNKS))]

    skip0 = (q != 0)
    gcC, b_gcC = P.sb("gcC", [128, 2, 256], BF16)
    gsC, b_gsC = P.sb("gsC", [128, 2, 256], BF16)
    cosC, b_cosC = P.sb("cosC", [128, 2, 256], BF16)
    nsinC, b_nsinC = P.sb("nsinC", [128, 2, 256], BF16)
    for tt, dd, bb in ((gcC, P.GCc, b_gcC), (gsC, P.GSc, b_gsC), (cosC, D_["cosC"], b_cosC), (nsinC, D_["nsinC"], b_nsinC)):
        S.dma("sync", lambda e: e.dma_start(out=tt[:], in_=dd), key=bb, writes=[bb])
    accs = [R.O_ring.next(), R.Z_ring.next()]
    for jc in range(0 if not skip0 else 2, 2):
        acc, b_acc = accs[jc]
        ops = []
        for nt in range(2):
            ops.append((gcC, b_gcC, cosC, b_cosC, nt))
            ops.append((gsC, b_gsC, nsinC, b_nsinC, nt))
        for ii, (g, b_g, tb, b_tb, nt) in enumerate(ops):
            S.op("pe", lambda e: e.matmul(acc[:, :256], lhsT=g[:, nt, jc * 128:(jc + 1) * 128], rhs=tb[:, nt, :], start=(ii == 0), stop=(ii == 3)),
                 reads=[b_g, b_tb], writes=[b_acc])
        S.op("act", lambda e: e.copy(out=mixT[:, 6 + jc, 0:256], in_=acc[:, :256]), reads=[b_acc], writes=[b_mix[0]])
    NG = 4
    cos_ring = P.ring("cosp", 2, [128, NG, 512], BF16)
    sin_ring = P.ring("sinp", 2, [128, NG, 512], BF16)
    gc_ring = P.ring("gcp", 2, [128, NG, 256], BF16)
    gs_ring = P.ring("gsp", 2, [128, NG, 256], BF16)
    cosL_d, nsinL_d = D_["cosL"][q], D_["nsinL"][q]
    for kc in range(4):
        ci = kc + 1
        t0, n = CHUNKS[ci]
        accs = [R.O_ring.next(), R.Z_ring.next()]
        for ng in range(64 // NG):
            cp, b_cp = cos_ring.next()
            sp, b_sp = sin_ring.next()
            gp, b_gp = gc_ring.next()
            hp, b_hp = gs_ring.next()
            n0 = ng * NG
            S.dma("sync", lambda e: e.dma_start(out=cp[:], in_=cosL_d[kc, :, n0:n0 + NG, :]), key=b_cp, writes=[b_cp])
            S.dma("sync", lambda e: e.dma_start(out=sp[:], in_=nsinL_d[kc, :, n0:n0 + NG, :]), key=b_sp, writes=[b_sp])
            S.dma("sync", lambda e: e.dma_start(out=gp[:], in_=P.GCs[:, n0:n0 + NG, :]), key=b_gp, writes=[b_gp])
            S.dma("sync", lambda e: e.dma_start(out=hp[:], in_=P.GSs[:, n0:n0 + NG, :]), key=b_hp, writes=[b_hp])
            for nt in range(NG):
                first = (ng == 0 and nt == 0)
                last = (ng == 64 // NG - 1 and nt == NG - 1)
                for jc in range(2):
                    acc, b_acc = accs[jc]
                    S.op("pe", lambda e: e.matmul(acc[:, :], lhsT=gp[:, nt, jc * 128:(jc + 1) * 128], rhs=cp[:, nt, :], start=first, stop=False),
                         reads=[b_gp, b_cp], writes=[b_acc])
                    S.op("pe", lambda e: e.matmul(acc[:, :], lhsT=hp[:, nt, jc * 128:(jc + 1) * 128], rhs=sp[:, nt, :], start=False, stop=last),
                         reads=[b_hp, b_sp], writes=[b_acc])
        for jc in range(2):
            acc, b_acc = accs[jc]
            S.op("act", lambda e: e.copy(out=mixT[:, 6 + jc, t0:t0 + 512], in_=acc[:, :]), reads=[b_acc], writes=[b_mix[ci]])

    Ksb, b_K = P.sb("Ksb", [128, 2, NKEY], BF16)
    Vsb, b_V = P.sb("Vsb", [128, NKT, 256], BF16)
    for h in range(2):
        S.dma("sync", lambda e: e.dma_start(out=Ksb[:, h, :], in_=P.Ks[h]), key=b_K, writes=[b_K])
    for hh in range(2):
        S.dma("sync", lambda e: e.dma_start(out=Vsb[:, hh * 33:(hh + 1) * 33, :], in_=P.Vs[:, hh * 33:(hh + 1) * 33, :]), key=b_V, writes=[b_V])
    q_ring = P.ring("qsb", 2, [128, T], BF16)
    scale = 128 ** -0.5
    for h in range(6):
        kvh = h // 3
        qs, b_qs = q_ring.next()
        S.dma("sync", lambda e: e.dma_start(out=qs[:], in_=P.Qs[q, h]), key=b_qs, writes=[b_qs])
        for ci, (t0, n) in enumerate(CHUNKS):
            if skip0 and ci == 0:
                continue
            nkt = 2 if ci == 0 else NKT
            ktiles = [(Ksb[:, kvh, j * 128:(j + 1) * 128], b_K) for j in range(nkt)]
            vtiles = [(Vsb[:, j, kvh * 128:(kvh + 1) * 128], b_V) for j in range(nkt)]
            attn_unit(P, R, qs[:, t0:t0 + n], b_qs, ktiles, vtiles, n, scale, P.biasm8[:, 0:1], mixT[:, h, t0:t0 + n], b_mix[ci])

    emit_outproj(P, mixT, b_mix, D_["w_out_even"][i], Xin[q], Xout[q], 16, R.S_ring, skip0=skip0)
    P.release(mk)


def phase_lam(P, l):
    S = P.S
    D_ = P.D
    mk = P.mark()
    i = l // 2
    lam_init = 0.8 - 0.6 * math.exp(-0.3 * l)
    lamv, b_lamv = P.sb("lamv", [128, 4, 64], F32)
    S.dma("sync", lambda e: e.dma_start(out=lamv[:], in_=D_["lamv"][i]), key=b_lamv, writes=[b_lamv])
    lp, b_lp = P.sb("lp", [128, 2, 64], F32)
    S.op("dve", lambda e: e.tensor_tensor(out=lp[:, 0, :], in0=lamv[:, 0, :], in1=lamv[:, 1, :], op=ALU.mult), reads=[b_lamv], writes=[b_lp])
    S.op("dve", lambda e: e.tensor_tensor(out=lp[:, 1, :], in0=lamv[:, 2, :], in1=lamv[:, 3, :], op=ALU.mult), reads=[b_lamv], writes=[b_lp])
    ls, b_ls = P.sb("ls", [128, 2], F32)
    S.op("dve", lambda e: e.reduce_sum(out=ls[:], in_=lp[:], axis=AX.X), reads=[b_lp], writes=[b_ls])
    le, b_le = P.sb("le", [128, 2], F32)
    S.op("act", lambda e: e.activation(out=le[:], in_=ls[:], func=AF.Exp), reads=[b_ls], writes=[b_le])
    b_n = P.b_lam
    S.op("dve", lambda e: e.tensor_tensor(out=P.nlam[:], in0=le[:, 1:2], in1=le[:, 0:1], op=ALU.subtract), reads=[b_le], writes=[b_n])
    S.op("dve", lambda e: e.tensor_scalar(out=P.nlam[:], in0=P.nlam[:], scalar1=-lam_init, scalar2=None, op0=ALU.add), reads=[b_n], writes=[b_n])
    hgr, b_hgr = P.sb("hgr", [128, 1], F32)
    S.dma("sync", lambda e: e.dma_start(out=hgr[:], in_=D_["hgain"][i]), key=b_hgr, writes=[b_hgr])
    S.op("dve", lambda e: e.tensor_scalar(out=P.hg[:], in0=hgr[:], scalar1=(1.0 - lam_init), scalar2=None, op0=ALU.mult), reads=[b_hgr], writes=[b_n])
    P.release(mk)


def phase_B_odd(P, l, q, Xin, Xout):
    S = P.S
    D_ = P.D
    mk = P.mark()
    i = l // 2
    R = attn_setup(P)
    skip0 = (q != 0)
    nlam, hg, b_n = P.nlam, P.hg, P.b_lam
    mixT, _ = P.sb("mixT", [128, KC, T], BF16)
    b_mix = [Buf("mix%d" % c) for c in range(len(CHUNKS))]
    k_ring = P.ring("ksb", 2, [128, NKEY], BF16)
    v_ring = P.ring("vsb", 2, [128, NKT, 128], BF16)
    q0_ring = P.ring("q0p", 2, [128, T], BF16)
    q1_ring = P.ring("q1p", 2, [128, T], BF16)
    for (qt, b_qt) in q0_ring.items:
        S.op("pool", lambda e: e.memset(qt[64:128, :], 0.0), writes=[b_qt])
    for (qt, b_qt) in q1_ring.items:
        S.op("pool", lambda e: e.memset(qt[0:64, :], 0.0), writes=[b_qt])
    a_ring = P.ring("am", 4, [128, 512], F32)
    o_ring = P.ring("ocomb", 2, [128, 512], F32)
    sq_ring = P.ring("sq", 2, [128, 512], BF16)
    rs_ring = P.ring("rs", 2, [128, 512], F32)
    scale = 64 ** -0.5
    for h in range(8):
        ks, b_ks = k_ring.next()
        vs, b_vs = v_ring.next()
        q0, b_q0 = q0_ring.next()
        q1, b_q1 = q1_ring.next()
        S.dma("sync", lambda e: e.dma_start(out=ks[:], in_=P.Ks[h]), key=b_ks, writes=[b_ks])
        for hh in range(2):
            S.dma("sync", lambda e: e.dma_start(out=vs[:, hh * 33:(hh + 1) * 33, :], in_=P.Vo[h, :, hh * 33:(hh + 1) * 33, :]), key=b_vs, writes=[b_vs])
        S.dma("sync", lambda e: e.dma_start(out=q0[0:64, :], in_=P.Qs[q, h, 0:64, :]), key=b_q0, writes=[b_q0])
        S.dma("sync", lambda e: e.dma_start(out=q1[64:128, :], in_=P.Qs[q, h, 64:128, :]), key=b_q1, writes=[b_q1])
        qm = [(q0, b_q0), (q1, b_q1)]
        for ci, (t0, n) in enumerate(CHUNKS):
            if skip0 and ci == 0:
                continue
            nkt = 2 if ci == 0 else NKT
            am = []
            for m in range(2):
                ktiles = [(ks[:, j * 128:(j + 1) * 128], b_ks) for j in range(nkt)]
                vtiles = [(vs[:, j, :], b_vs) for j in range(nkt)]
                a, b_a = a_ring.next()
                attn_unit(P, R, qm[m][0][:, t0:t0 + n], qm[m][1], ktiles, vtiles, n, scale, P.bias0[:, 0:1], a[:, :n], b_a)
                am.append((a, b_a))
            oc, b_oc = o_ring.next()
            S.op("dve", lambda e: e.scalar_tensor_tensor(out=oc[:, :n], in0=am[1][0][:, :n], scalar=nlam[:, 0:1], in1=am[0][0][:, :n],
                                                         op0=ALU.mult, op1=ALU.add), reads=[am[0][1], am[1][1], b_n], writes=[b_oc])
            sq, b_sq = sq_ring.next()
            S.op("act", lambda e: e.activation(out=sq[:, :n], in_=oc[:, :n], func=AF.Square), reads=[b_oc], writes=[b_sq])
            ss, b_ss = R.S_ring.next()
            S.op("pe", lambda e: e.matmul(ss[:, :n], lhsT=P.ones[:], rhs=sq[:, :n], start=True, stop=True), reads=[b_sq, P.b_ones], writes=[b_ss])
            rs, b_rs = rs_ring.next()
            emit_rstd(P, ss, b_ss, n, 1.0 / 128, rs, b_rs)
            S.op("dve", lambda e: e.scalar_tensor_tensor(out=mixT[:, h, t0:t0 + n], in0=oc[:, :n], scalar=hg[:, 0:1], in1=rs[:, :n],
                                                         op0=ALU.mult, op1=ALU.mult), reads=[b_oc, b_rs, b_n], writes=[b_mix[ci]])

    emit_outproj(P, mixT, b_mix, D_["w_out_odd"][i], Xin[q], Xout[q], 16, R.S_ring, skip0=skip0)
    P.release(mk)


def phase_C(P, l, q, Xin, Xout, last):
    S = P.S
    D_ = P.D
    mk = P.mark()
    psA = Ring([P.bank[3], P.bank[4], P.bank[5], P.bank[6]])
    psD = Ring([P.bank[0], P.bank[1]])
    psG = P.bank[2]
    modT, b_mod = P.modT, P.b_mod
    skip0 = (q != 0) or last
    xs = XSrc(P, Xin[q], resident=True)
    xT, b_x = xs.xT, xs.bufs
    hT, _ = P.sb("hT", [128, KC, T], BF16)
    b_h = [Buf("h%d" % c) for c in range(len(CHUNKS))]
    sq_ring = P.ring("sq", 2, [128, 512], BF16)
    rs_ring = P.ring("rs", 2, [128, 512], F32)
    tmp_ring = P.ring("tmp", 2, [128, 512], F32)
    emit_norm_mod(P, xs, hT, b_h, 24, 32, psD, sq_ring, rs_ring, tmp_ring, skip0=skip0)
    gT, b_gT = P.sb("gT", [16, T], BF16)
    mk2 = P.mark()

    wr, b_wr = P.sb("wr", [128, KC, 20], BF16)
    wrv = D_["wr"][l].rearrange("(k p) n -> p k n", p=128)
    for k in range(KC):
        S.dma("pool", lambda e: e.dma_start(out=wr[:, k, :], in_=wrv[:, k, :]), key=b_wr, writes=[b_wr])
    br, b_br = P.sb("br", [128, 20], F32)
    S.dma("sync", lambda e: e.dma_start(out=br[:], in_=D_["br"][l]), key=b_br, writes=[b_br])
    L, b_L = P.sb("L", [128, NT, 20], F32)
    for tt in range(NT):
        if skip0 and tt < 2:
            continue
        ci = 0 if tt < 2 else 1 + (tt - 2) // 4
        pl, b_pl = psD.next()
        for k in range(KC):
            S.op("pe", lambda e: e.matmul(pl[:, 0:20], lhsT=hT[:, k, tt * 128:(tt + 1) * 128], rhs=wr[:, k, :], start=(k == 0), stop=(k == KC - 1)),
                 reads=[b_wr, b_h[ci]], writes=[b_pl])
        S.op("dve", lambda e: e.tensor_tensor(out=L[:, tt, :], in0=pl[:, 0:20], in1=br[:], op=ALU.add), reads=[b_pl, b_br], writes=[b_L])

    cnt = [0]

    def tmp():
        cnt[0] += 1
        return P.sb("g%d" % cnt[0], [128, NT], F32)

    def tt_(out, a, b, op, rd, wr_):
        S.op("dve", lambda e: e.tensor_tensor(out=out, in0=a, in1=b, op=op), reads=rd, writes=wr_)

    gl = [L[:, :, g] for g in range(4)]
    m01, b_m01 = tmp()
    m23, b_m23 = tmp()
    gmax, b_gmax = tmp()
    tt_(m01[:], gl[0], gl[1], ALU.max, [b_L], [b_m01])
    tt_(m23[:], gl[2], gl[3], ALU.max, [b_L], [b_m23])
    tt_(gmax[:], m01[:], m23[:], ALU.max, [b_m01, b_m23], [b_gmax])
    masks = []
    sumexp, b_se = tmp()
    for g in range(4):
        dg, b_dg = tmp()
        tt_(dg[:], gl[g], gmax[:], ALU.subtract, [b_L, b_gmax], [b_dg])
        eg, b_eg = tmp()
        S.op("act", lambda e: e.activation(out=eg[:], in_=dg[:], func=AF.Exp), reads=[b_dg], writes=[b_eg])
        if g == 0:
            S.op("dve", lambda e: e.tensor_copy(out=sumexp[:], in_=eg[:]), reads=[b_eg], writes=[b_se])
        else:
            tt_(sumexp[:], sumexp[:], eg[:], ALU.add, [b_se, b_eg], [b_se])
        mk_, b_mk = tmp()
        tt_(mk_[:], gl[g], gmax[:], ALU.is_equal, [b_L, b_gmax], [b_mk])
        masks.append((mk_, b_mk))
    ggate, b_gg = tmp()
    S.op("dve", lambda e: e.reciprocal(out=ggate[:], in_=sumexp[:]), reads=[b_se], writes=[b_gg])
    esel = []
    for j in range(4):
        es, b_es = tmp()
        pr_, b_pr_ = tmp()
        for g in range(4):
            mk_, b_mk = masks[g]
            if g == 0:
                tt_(es[:], L[:, :, 4 + g * 4 + j], mk_[:], ALU.mult, [b_L, b_mk], [b_es])
            else:
                tt_(pr_[:], L[:, :, 4 + g * 4 + j], mk_[:], ALU.mult, [b_L, b_mk], [b_pr_])
                tt_(es[:], es[:], pr_[:], ALU.add, [b_es, b_pr_], [b_es])
        esel.append((es, b_es))

    def max4(items):
        a, b_a = tmp()
        b, b_b = tmp()
        c, b_c = tmp()
        tt_(a[:], items[0][0][:], items[1][0][:], ALU.max, [items[0][1], items[1][1]], [b_a])
        tt_(b[:], items[2][0][:], items[3][0][:], ALU.max, [items[2][1], items[3][1]], [b_b])
        tt_(c[:], a[:], b[:], ALU.max, [b_a, b_b], [b_c])
        return c, b_c

    e1, b_e1 = max4(esel)
    m1 = []
    esel2 = []
    for j in range(4):
        mk_, b_mk = tmp()
        tt_(mk_[:], esel[j][0][:], e1[:], ALU.is_equal, [esel[j][1], b_e1], [b_mk])
        m1.append((mk_, b_mk))
        e2_, b_e2_ = tmp()
        S.op("dve", lambda e: e.scalar_tensor_tensor(out=e2_[:], in0=mk_[:], scalar=-1e30, in1=esel[j][0][:], op0=ALU.mult, op1=ALU.add),
             reads=[b_mk, esel[j][1]], writes=[b_e2_])
        esel2.append((e2_, b_e2_))
    e2, b_e2 = max4(esel2)
    m2 = []
    for j in range(4):
        mk_, b_mk = tmp()
        tt_(mk_[:], esel2[j][0][:], e2[:], ALU.is_equal, [esel2[j][1], b_e2], [b_mk])
        m2.append((mk_, b_mk))
    dd, b_dd = tmp()
    tt_(dd[:], e2[:], e1[:], ALU.subtract, [b_e2, b_e1], [b_dd])
    rr, b_rr = tmp()
    S.op("act", lambda e: e.activation(out=rr[:], in_=dd[:], func=AF.Exp), reads=[b_dd], writes=[b_rr])
    den, b_den = tmp()
    S.op("dve", lambda e: e.tensor_scalar(out=den[:], in0=rr[:], scalar1=1.0, scalar2=None, op0=ALU.add), reads=[b_rr], writes=[b_den])
    S.op("dve", lambda e: e.reciprocal(out=den[:], in_=den[:]), reads=[b_den], writes=[b_den])
    w1, b_w1 = tmp()
    tt_(w1[:], ggate[:], den[:], ALU.mult, [b_gg, b_den], [b_w1])
    w2, b_w2 = tmp()
    tt_(w2[:], w1[:], rr[:], ALU.mult, [b_w1, b_rr], [b_w2])
    G, b_G = P.sb("G", [128, NT, 16], F32)
    for j in range(4):
        ew, b_ew = tmp()
        e2w, b_e2w = tmp()
        tt_(ew[:], m1[j][0][:], w1[:], ALU.mult, [m1[j][1], b_w1], [b_ew])
        tt_(e2w[:], m2[j][0][:], w2[:], ALU.mult, [m2[j][1], b_w2], [b_e2w])
        tt_(ew[:], ew[:], e2w[:], ALU.add, [b_ew, b_e2w], [b_ew])
        for g in range(4):
            tt_(G[:, :, g * 4 + j], masks[g][0][:], ew[:], ALU.mult, [masks[g][1], b_ew], [b_G])

    for tt in range(NT):
        if skip0 and tt < 2:
            continue
        pt_, b_pt_ = psD.next()
        S.op("pe", lambda e: e.matmul(pt_[0:16, 0:128], lhsT=G[:, tt, :], rhs=P.ident[:], start=True, stop=True), reads=[b_G, P.b_const], writes=[b_pt_])
        S.op("act", lambda e: e.copy(out=gT[:, tt * 128:(tt + 1) * 128], in_=pt_[0:16, 0:128]), reads=[b_pt_], writes=[b_gT])

    P.release(mk2)
    wg_ring = P.ring("wg", 2, [128, KC, 512], BF16)
    wu_ring = P.ring("wu", 2, [128, KC, 512], BF16)
    wd_ring = P.ring("wd", 2, [128, 4, D], BF16)
    sil_ring = P.ring("sil", 2, [128, 512], F32)
    us_ring = P.ring("us", 2, [128, 512], F32)
    a_ring = P.ring("aT", 8, [128, 512], BF16)
    wts = {}

    def load_expert(ex):
        wg, b_wg = wg_ring.next()
        wu, b_wu = wu_ring.next()
        wd, b_wd = wd_ring.next()
        wgv = D_["w_gate"][l, ex].rearrange("(k p) n -> p k n", p=128)
        wuv = D_["w_up"][l, ex].rearrange("(k p) n -> p k n", p=128)
        wdv = D_["w_down"][l, ex].rearrange("(k p) n -> p k n", p=128)
        for k in range(KC):
            S.dma("pool", lambda e: e.dma_start(out=wg[:, k, :], in_=wgv[:, k, :]), key=b_wg, writes=[b_wg])
        for k in range(KC):
            S.dma("pool", lambda e: e.dma_start(out=wu[:, k, :], in_=wuv[:, k, :]), key=b_wu, writes=[b_wu])
        for k in range(4):
            S.dma("pool", lambda e: e.dma_start(out=wd[:, k, :], in_=wdv[:, k, :]), key=b_wd, writes=[b_wd])
        wts[ex] = (wg, b_wg, wu, b_wu, wd, b_wd)

    def emit_GU(ex, ci):
        t0, n = CHUNKS[ci]
        wg, b_wg, wu, b_wu, wd, b_wd = wts[ex]
        pgb, b_pgb = psG
        S.op("pe", lambda e: e.matmul(pgb[:, :n], lhsT=P.sel[:, ex, :], rhs=gT[:, t0:t0 + n], start=True, stop=True), reads=[P.b_const, b_gT], writes=[b_pgb])
        As = []
        for f in range(4):
            pG, b_pG = psA.next()
            pU, b_pU = psA.next()
            for k in range(KC):
                S.op("pe", lambda e: e.matmul(pG[:, :n], lhsT=wg[:, k, f * 128:(f + 1) * 128], rhs=hT[:, k, t0:t0 + n], start=(k == 0), stop=(k == KC - 1)),
                     reads=[b_wg, b_h[ci]], writes=[b_pG])
            for k in range(KC):
                S.op("pe", lambda e: e.matmul(pU[:, :n], lhsT=wu[:, k, f * 128:(f + 1) * 128], rhs=hT[:, k, t0:t0 + n], start=(k == 0), stop=(k == KC - 1)),
                     reads=[b_wu, b_h[ci]], writes=[b_pU])
            sl, b_sl = sil_ring.next()
            S.op("act", lambda e: e.activation(out=sl[:, :n], in_=pG[:, :n], func=AF.Silu), reads=[b_pG], writes=[b_sl])
            us, b_us = us_ring.next()
            S.op("dve", lambda e: e.tensor_tensor(out=us[:, :n], in0=pU[:, :n], in1=sl[:, :n], op=ALU.mult), reads=[b_pU, b_sl], writes=[b_us])
            aT, b_aT = a_ring.next()
            S.op("dve", lambda e: e.tensor_tensor(out=aT[:, :n], in0=us[:, :n], in1=pgb[:, :n], op=ALU.mult), reads=[b_us, b_pgb], writes=[b_aT])
            As.append((aT, b_aT))
        return As

    def emit_D(ex, ci, As):
        t0, n = CHUNKS[ci]
        t = 0 if ci == 0 else 1
        wg, b_wg, wu, b_wu, wd, b_wd = wts[ex]
        for m in range(KC):
            pD, b_pD = psD.next()
            for f in range(4):
                aT_f = As[f][0]
                S.op("pe", lambda e: e.matmul(pD[:, :n], lhsT=wd[:, f, m * 128:(m + 1) * 128], rhs=aT_f[:, :n], start=(f == 0), stop=(f == 3)),
                     reads=[b_wd, As[f][1]], writes=[b_pD])
            S.op("dve", lambda e: e.scalar_tensor_tensor(out=xT[:, m, t0:t0 + n], in0=pD[:, :n], scalar=modT[:, 40 + m, t:t + 1], in1=xT[:, m, t0:t0 + n],
                                                         op0=ALU.mult, op1=ALU.add), reads=[b_pD, b_mod, b_x[ci]], writes=[b_x[ci]])

    items = [(ex, ci) for ex in range(16) for ci in range(len(CHUNKS)) if not (skip0 and ci == 0)]
    load_expert(0)
    load_expert(1)
    cur = emit_GU(*items[0])
    for k, (ex, ci) in enumerate(items):
        nxt = None
        if k + 1 < len(items):
            ex2, ci2 = items[k + 1]
            nxt = emit_GU(ex2, ci2)
        emit_D(ex, ci, cur)
        if k + 1 < len(items) and items[k + 1][0] != ex and ex + 2 < 16:
            load_expert(ex + 2)
        cur = nxt

    if not last:
        for ci, (t0, n) in enumerate(CHUNKS):
            if skip0 and ci == 0:
                continue
            S.dma("sync", lambda e: e.dma_start(out=Xout[q][:, :, t0:t0 + n], in_=xT[:, :, t0:t0 + n]), key=b_x[ci], reads=[b_x[ci]])
    else:
        fg, b_fg = P.sb("fg", [128, KC], F32)
        S.dma("sync", lambda e: e.dma_start(out=fg[:], in_=D_["fgain"]), key=b_fg, writes=[b_fg])
        oring = tmp_ring
        for ci, (t0, n) in enumerate(CHUNKS):
            if ci == 0:
                continue
            ss, b_ss = psD.next()
            for k in range(KC):
                sq, b_sq = sq_ring.next()
                S.op("act", lambda e: e.activation(out=sq[:, :n], in_=xT[:, k, t0:t0 + n], func=AF.Square), reads=[b_x[ci]], writes=[b_sq])
                S.op("pe", lambda e: e.matmul(ss[:, :n], lhsT=P.ones[:], rhs=sq[:, :n], start=(k == 0), stop=(k == KC - 1)), reads=[b_sq, P.b_ones], writes=[b_ss])
            rs, b_rs = rs_ring.next()
            emit_rstd(P, ss, b_ss, n, 1.0 / D, rs, b_rs)
            for k in range(KC):
                fo, b_fo = oring.next()
                S.op("dve", lambda e: e.scalar_tensor_tensor(out=fo[:, :n], in0=xT[:, k, t0:t0 + n], scalar=fg[:, k:k + 1], in1=rs[:, :n],
                                                             op0=ALU.mult, op1=ALU.mult), reads=[b_x[ci], b_rs, b_fg], writes=[b_fo])
                S.dma("sync", lambda e: e.dma_start(out=P.out_d[q, :, k, t0 - CTX:t0 - CTX + n], in_=fo[:, :n]), key=b_fo, reads=[b_fo], is_out=True)
    P.release(mk)


NQ = 4


def build_fused(depth=DEPTH):
    P = Prog()
    S = P.S
    D_ = {}
    P.D = D_
    D_["xT"] = P.din("xT", [NQ, 128, KC, T], F32)
    D_["cvec"] = P.din("cvec", [128, KC, 2], F32)
    D_["w_ada"] = P.din("w_ada", [DEPTH, D, 6 * D], F32)
    D_["b_ada"] = P.din("b_ada", [DEPTH, 128, 48], F32)
    D_["w_in_even"] = P.din("w_in_even", [2, D, 1536], F32)
    D_["w_in_odd"] = P.din("w_in_odd", [2, D, 3072], F32)
    D_["w_out_even"] = P.din("w_out_even", [2, D, D], F32)
    D_["w_out_odd"] = P.din("w_out_odd", [2, D, D], F32)
    D_["gains"] = P.din("gains", [2, 128, 4], F32)
    D_["lamv"] = P.din("lamv", [2, 128, 4, 64], F32)
    D_["hgain"] = P.din("hgain", [2, 128, 1], F32)
    D_["wr"] = P.din("wr", [DEPTH, D, 20], F32)
    D_["br"] = P.din("br", [DEPTH, 128, 20], F32)
    D_["w_gate"] = P.din("w_gate", [DEPTH, 16, D, 512], F32)
    D_["w_up"] = P.din("w_up", [DEPTH, 16, D, 512], F32)
    D_["w_down"] = P.din("w_down", [DEPTH, 16, 512, D], F32)
    D_["fgain"] = P.din("fgain", [128, KC], F32)
    for nm in ("ropecos128", "ropesin128", "ropecos64", "ropesin64"):
        D_[nm] = P.din(nm, [NQ, 128, T], F32)
    cst_d = P.din("consts_bf", [128, 6, 128], BF16)
    sel_d = P.din("sel", [16, 16, 128], BF16)
    id_d = P.din("ident", [128, 128], F32)
    D_["cosL"] = P.din("cosL", [NQ, 4, 128, 64, 512], BF16)
    D_["nsinL"] = P.din("nsinL", [NQ, 4, 128, 64, 512], BF16)
    D_["cosC"] = P.din("cosC", [128, 2, 256], BF16)
    D_["nsinC"] = P.din("nsinC", [128, 2, 256], BF16)
    P.out_d = P.dout("out", [NQ, 128, KC, TL], F32)
    X1 = P.dscr("X1", [NQ, 128, KC, T], F32)
    X2 = P.dscr("X2", [NQ, 128, KC, T], F32)
    X3 = P.dscr("X3", [NQ, 128, KC, T], F32)
    P.Qs = P.dscr("Qs", [NQ, 8, 128, T], BF16)
    P.Ks = P.dscr("Ks", [8, 128, NKEY], BF16)
    P.Vs = P.dscr("Vs", [128, NKT, 256], BF16)
    P.Vo = P.dscr("Vo", [8, 128, NKT, 128], BF16)
    P.GCs = P.dscr("GCs", [128, 64, 256], BF16)
    P.GSs = P.dscr("GSs", [128, 64, 256], BF16)
    P.GCc = P.dscr("GCc", [128, 2, 256], BF16)
    P.GSc = P.dscr("GSc", [128, 2, 256], BF16)

    P.bank = [P.ps("bank%d" % b) for b in range(8)]
    P.ones, P.b_ones = P.sb("ones", [128, 128], BF16)
    S.op("pool", lambda e: e.memset(P.ones[:], 1.0), writes=[P.b_ones])
    P.ones32, _ = P.sb("ones32", [128, 128], F32)
    S.op("pool", lambda e: e.memset(P.ones32[:], 1.0), writes=[P.b_ones])
    P.epsc, P.b_eps = P.sb("epsc", [128, 1], F32)
    S.op("pool", lambda e: e.memset(P.epsc[:], EPS), writes=[P.b_eps])
    P.b_const = Buf("const")
    P.biasm8, _ = P.sb("biasm8", [128, 1], F32)
    S.op("pool", lambda e: e.memset(P.biasm8[:], -8.0), writes=[P.b_const])
    P.bias0, _ = P.sb("bias0", [128, 1], F32)
    S.op("pool", lambda e: e.memset(P.bias0[:], 0.0), writes=[P.b_const])
    cst, _ = P.sb("cst", [128, 6, 128], BF16)
    S.dma("sync", lambda e: e.dma_start(out=cst[:], in_=cst_d), key=P.b_const, writes=[P.b_const])
    P.PT128, P.PT64, P.bdc, P.bds, P.bdo = cst[:, 0, :], cst[:, 1, :], cst[:, 2, :], cst[:, 3, :], cst[:, 4, :]
    P.sel, _ = P.sb("sel", [16, 16, 128], BF16)
    S.dma("sync", lambda e: e.dma_start(out=P.sel[:], in_=sel_d), key=P.b_const, writes=[P.b_const])
    P.ident, _ = P.sb("ident", [128, 128], F32)
    S.dma("sync", lambda e: e.dma_start(out=P.ident[:], in_=id_d), key=P.b_const, writes=[P.b_const])
    P.modT, P.b_mod = P.sb("modT", [128, 48, 2], F32)
    P.scp, _ = P.sb("scp", [128, 48, 2], F32)
    P.nlam, P.b_lam = P.sb("nlam", [128, 1], F32)
    P.hg, _ = P.sb("hg", [128, 1], F32)
    cv, b_cv = P.sb("cv", [128, KC, 2], F32)
    S.dma("sync", lambda e: e.dma_start(out=cv[:], in_=D_["cvec"]), key=b_cv, writes=[b_cv])
    P.cvb, P.b_cvb = P.sb("cvb", [128, KC, 2], BF16)
    S.op("act", lambda e: e.activation(out=P.cvb[:], in_=cv[:], func=AF.Silu), reads=[b_cv], writes=[P.b_cvb])
    S.fence()

    Xin = D_["xT"]
    for l in range(depth):
        last = (l == DEPTH - 1)
        even = (l % 2 == 0)
        Xmid = X1
        Xout = X2 if even else X3
        phase_mod(P, l)
        if not even:
            phase_lam(P, l)
        for q in range(NQ):
            phase_A(P, l, q, Xin)
        for q in range(NQ):
            if even:
                phase_B_even(P, l, q, Xin, Xmid)
            else:
                phase_B_odd(P, l, q, Xin, Xmid)
        for q in range(NQ):
            phase_C(P, l, q, Xmid, Xout, last)
        Xin = Xout
    if depth < DEPTH:
        xdbg = P.dout("xdbg", [NQ, 128, KC, T], F32)
        bdbg = Buf("dbg")
        for q in range(NQ):
            S.dma("sync", lambda e: e.dma_start(out=xdbg[q], in_=Xin[q]), key=bdbg, writes=[bdbg], is_out=True)
    return P.finish()


def _to_fm(xtok):
    return np.ascontiguousarray(xtok.reshape(xtok.shape[0], KC, 128).transpose(2, 1, 0))


def _from_fm(xfm):
    return np.ascontiguousarray(xfm.transpose(2, 1, 0).reshape(xfm.shape[2], KC * 128))


def _rope_tables(dim, core_q):
    half = dim // 2
    nf = half // 2
    inv = (10000.0 ** (-np.arange(0, half, 2, dtype=np.float32) / np.float32(half))).astype(np.float32)
    tok = np.arange(core_q * TL, (core_q + 1) * TL)
    row = (tok // 64).astype(np.float32)
    col = (tok % 64).astype(np.float32)
    ang_r = row[:, None] * inv[None, :]
    ang_c = col[:, None] * inv[None, :]
    cosT = np.ones((128, T), np.float32)
    sinT = np.zeros((128, T), np.float32)
    d = np.arange(128)
    u = d % dim
    is_col = u >= half
    fidx = u % nf
    cr, sr, cc, sc = np.cos(ang_r), np.sin(ang_r), np.cos(ang_c), np.sin(ang_c)
    cosT[:, CTX:] = np.where(is_col[:, None], cc[:, fidx].T, cr[:, fidx].T)
    sinT[:, CTX:] = np.where(is_col[:, None], sc[:, fidx].T, sr[:, fidx].T)
    PT = np.zeros((128, 128), np.float32)
    for m in range(128):
        if (m % half) < nf:
            PT[m + nf, m] = -1.0
        else:
            PT[m - nf, m] = 1.0
    return cosT, sinT, PT.astype(NPBF)


_CACHE = {}


def _dft_tables():
    if "dft" in _CACHE:
        return _CACHE["dft"]
    n = np.arange(SEQ, dtype=np.int64)
    scl = 1.0 / math.sqrt(SEQ * 64)
    cosL = np.empty((NQ, 4, 128, 64, 512), NPBF)
    nsinL = np.empty((NQ, 4, 128, 64, 512), NPBF)
    for q in range(NQ):
        k = np.arange(q * TL, (q + 1) * TL, dtype=np.int64)
        ang = (2.0 * np.pi / SEQ) * ((n[:, None] * k[None, :]) % SEQ).astype(np.float64)
        c = (np.cos(ang) * scl).astype(np.float32).astype(NPBF)
        s = (-np.sin(ang) * scl).astype(np.float32).astype(NPBF)
        cosL[q] = c.reshape(64, 128, 4, 512).transpose(2, 1, 0, 3)
        nsinL[q] = s.reshape(64, 128, 4, 512).transpose(2, 1, 0, 3)
    nn = np.arange(CTX, dtype=np.int64)
    ang = (2.0 * np.pi / CTX) * ((nn[:, None] * nn[None, :]) % CTX).astype(np.float64)
    sc2 = 1.0 / math.sqrt(CTX * 64)
    cC = np.ascontiguousarray((np.cos(ang) * sc2).astype(np.float32).astype(NPBF).reshape(2, 128, CTX).transpose(1, 0, 2))
    sC = np.ascontiguousarray((-np.sin(ang) * sc2).astype(np.float32).astype(NPBF).reshape(2, 128, CTX).transpose(1, 0, 2))
    cc = np.arange(64)
    a64 = (2.0 * np.pi / 64) * ((cc[:, None] * cc[None, :]) % 64)
    bdc = np.zeros((128, 128), np.float32)
    bds = np.zeros((128, 128), np.float32)
    bdo = np.zeros((128, 128), np.float32)
    for g in range(2):
        bdc[g * 64:(g + 1) * 64, g * 64:(g + 1) * 64] = np.cos(a64)
        bds[g * 64:(g + 1) * 64, g * 64:(g + 1) * 64] = np.sin(a64)
        bdo[g * 64:(g + 1) * 64, g * 64:(g + 1) * 64] = 1.0
    _CACHE["dft"] = (cosL, nsinL, cC, sC, bdc.astype(NPBF), bds.astype(NPBF), bdo.astype(NPBF))
    return _CACHE["dft"]


def _host_inputs(x, c, ctx, c_ctx, w_ada, b_ada, w_in_even, w_out_even, a_q_gain, a_k_gain, b_gain,
                 w_in_odd, w_out_odd, c_lambda_q1, c_lambda_k1, c_lambda_q2, c_lambda_k2, c_head_gain,
                 w_group, b_group, w_router, b_router, w_gate, w_up, w_down, final_gain):
    f32 = np.float32
    A = lambda a: np.ascontiguousarray(np.asarray(a, f32))
    x, ctx, c, c_ctx = A(x), A(ctx), A(c), A(c_ctx)
    cosL, nsinL, cC, sC, bdc, bds, bdo = _dft_tables()
    r128 = [_rope_tables(128, q) for q in range(NQ)]
    r64 = [_rope_tables(64, q) for q in range(NQ)]
    consts_bf = np.ascontiguousarray(np.stack([r128[0][2], r64[0][2], bdc, bds, bdo, np.eye(128, dtype=f32).astype(NPBF)], axis=1))
    sel = np.zeros((16, 16, 128), f32)
    for e in range(16):
        sel[e, e, :] = 1.0
    shared = {
        "w_ada": A(w_ada), "b_ada": np.ascontiguousarray(A(b_ada).reshape(DEPTH, 48, 128).transpose(0, 2, 1)),
        "w_in_even": A(w_in_even), "w_in_odd": A(w_in_odd), "w_out_even": A(w_out_even), "w_out_odd": A(w_out_odd),
        "gains": np.ascontiguousarray(np.stack([np.concatenate([A(a_q_gain)[i][:, None], A(a_k_gain)[i][:, None], A(b_gain)[i].reshape(2, 128).T], axis=1)
                                                for i in range(2)], axis=0)),
        "lamv": np.ascontiguousarray(np.stack([np.broadcast_to(np.stack([A(a_)[i] for a_ in (c_lambda_q1, c_lambda_k1, c_lambda_q2, c_lambda_k2)], 0)[None], (128, 4, 64))
                                               for i in range(2)], axis=0)),
        "hgain": np.ascontiguousarray(A(c_head_gain)[:, :, None]),
        "wr": np.ascontiguousarray(np.concatenate([A(w_group), A(w_router)], axis=2)),
        "br": np.ascontiguousarray(np.broadcast_to(np.concatenate([A(b_group), A(b_router)], axis=1)[:, None, :], (DEPTH, 128, 20))),
        "w_gate": A(w_gate), "w_up": A(w_up), "w_down": A(w_down),
        "fgain": np.ascontiguousarray(A(final_gain).reshape(KC, 128).T),
        "ropecos128": np.ascontiguousarray(np.stack([r[0] for r in r128])), "ropesin128": np.ascontiguousarray(np.stack([r[1] for r in r128])),
        "ropecos64": np.ascontiguousarray(np.stack([r[0] for r in r64])), "ropesin64": np.ascontiguousarray(np.stack([r[1] for r in r64])),
        "consts_bf": consts_bf, "sel": sel.astype(NPBF), "ident": np.eye(128, dtype=f32),
        "cosL": cosL, "nsinL": nsinL, "cosC": cC, "nsinC": sC,
    }
    per_batch = []
    for b in range(2):
        xq = np.stack([_to_fm(np.concatenate([ctx[b], x[b, q * TL:(q + 1) * TL]], axis=0)) for q in range(NQ)], axis=0)
        cvec = np.ascontiguousarray(np.stack([c_ctx, c[b]], axis=-1).reshape(KC, 128, 2).transpose(1, 0, 2))
        m = dict(shared)
        m["xT"] = xq
        m["cvec"] = cvec
        per_batch.append(m)
    return [per_batch[core // 4] for core in range(NCORES)]


def kernel(**inputs):
    if "nc" not in _CACHE:
        _CACHE["nc"] = build_fused()
    maps = _host_inputs(**inputs)
    res = run_bass_kernel_spmd(_CACHE["nc"], maps, core_ids=list(range(NCORES))).results
    out = np.zeros((2, SEQ, D), np.float32)
    for b in range(2):
        o = res[4 * b]["out"]
        for q in range(NQ):
            out[b, q * TL:(q + 1) * TL] = _from_fm(o[q])
    return out
```

```python
import contextlib
import math
import numpy as np
import ml_dtypes
import concourse.bass as bass
import concourse.mybir as mybir
from concourse.bass_utils import run_bass_kernel_spmd

F32 = mybir.dt.float32
BF16 = mybir.dt.bfloat16
AF = mybir.ActivationFunctionType
ALU = mybir.AluOpType
AX = mybir.AxisListType
NPBF = ml_dtypes.bfloat16

NCORES = 8
D = 1024
KC = 8
SEQ = 8192
CTX = 256
TL = 2048
T = CTX + TL
NT = T // 128
CHUNKS = [(0, 256), (256, 512), (768, 512), (1280, 512), (1792, 512)]
NKEY = CTX + SEQ
NKT = NKEY // 128
EPS = 1e-6
DEPTH = 4


class Buf:
    __slots__ = ("name", "last_w", "readers")

    def __init__(self, name=""):
        self.name = name
        self.last_w = None
        self.readers = []


class _Rec:
    def __init__(self):
        self.call = None

    def __getattr__(self, name):
        def f(*a, **kw):
            self.call = (name, a, kw)
        return f


class Op:
    __slots__ = ("eng", "fn", "deps", "is_dma", "key", "cnt", "need_inc", "dma_val")

    def __init__(self, eng, fn, is_dma=False, key=None):
        self.eng = eng
        r = _Rec()
        fn(r)
        name, a, kw = r.call
        self.fn = lambda e: getattr(e, name)(*a, **kw)
        self.deps = []
        self.is_dma = is_dma
        self.key = key
        self.cnt = None
        self.need_inc = False
        self.dma_val = None


class Sched:
    ENGS = ("sync", "act", "dve", "pool", "pe")

    def __init__(self, nc):
        self.nc = nc
        self.q = {e: [] for e in self.ENGS}
        self.slot_of = {}
        self.slot_cnt = []
        self.slot_last = []
        self.out_dmas = []

    def fence(self):
        tails = [self.q[e][-1] for e in self.ENGS if self.q[e] and not self.q[e][-1].is_dma]
        for e in self.ENGS:
            for o in reversed(self.q[e]):
                if not o.is_dma:
                    if o not in tails:
                        tails.append(o)
                    break
        tails += [o for o in self.slot_last if o is not None]
        for e in self.ENGS:
            o = Op(e, lambda x: x.nop())
            for d in tails:
                if d.is_dma or d.eng != e:
                    o.deps.append(d)
                    if not d.is_dma:
                        d.need_inc = True
            self.q[e].append(o)
        self.slot_of = {}

    def _add(self, op, reads, writes):
        deps = []
        for b in reads:
            if b.last_w is not None:
                deps.append(("raw", b.last_w))
        for b in writes:
            if b.last_w is not None:
                deps.append(("waw", b.last_w))
            for r in b.readers:
                deps.append(("war", r))
        for kind, d in deps:
            if d is op:
                continue
            if d.is_dma:
                op.deps.append(d)
            elif d.eng == op.eng and not op.is_dma:
                if kind == "raw" and op.eng != "pe":
                    op.deps.append(d)
                    d.need_inc = True
            else:
                op.deps.append(d)
                d.need_inc = True
        for b in reads:
            b.readers.append(op)
        for b in writes:
            b.last_w = op
            b.readers = []
        self.q[op.eng].append(op)
        return op

    def op(self, eng, fn, reads=(), writes=()):
        return self._add(Op(eng, fn), list(reads), list(writes))

    def dma(self, eng, fn, key, reads=(), writes=(), is_out=False):
        if key not in self.slot_of:
            self.slot_of[key] = len(self.slot_of)
            if len(self.slot_cnt) < len(self.slot_of):
                self.slot_cnt.append(0)
                self.slot_last.append(None)
        slot = self.slot_of[key]
        o = Op(eng, fn, is_dma=True, key=slot)
        self.slot_cnt[slot] += 1
        o.dma_val = 16 * self.slot_cnt[slot]
        self.slot_last[slot] = o
        self._add(o, list(reads), list(writes))
        if is_out:
            self.out_dmas.append(o)
        return o

    def emit(self):
        nc = self.nc
        with contextlib.ExitStack() as st:
            esem = {e: st.enter_context(nc.semaphore("s_" + e)) for e in self.ENGS}
            dsem = {i: st.enter_context(nc.semaphore("d_%d" % i)) for i in range(len(self.slot_cnt))}
            for e in self.ENGS:
                c = 0
                for o in self.q[e]:
                    if (not o.is_dma) and o.need_inc:
                        c += 1
                        o.cnt = c
            block = st.enter_context(nc.Block())
            finals = {}
            for d in self.out_dmas:
                finals[d.key] = max(finals.get(d.key, 0), d.dma_val)

            def run(ename, eng):
                waited = {}
                for o in self.q[ename]:
                    for d in o.deps:
                        if d.is_dma:
                            s, v = dsem[d.key], d.dma_val
                        else:
                            s, v = esem[d.eng], d.cnt
                        kk = id(s)
                        if waited.get(kk, 0) >= v:
                            continue
                        waited[kk] = v
                        eng.wait_ge(s, v)
                    ins = o.fn(eng)
                    if o.is_dma:
                        ins.then_inc(dsem[o.key], 16)
                    elif o.need_inc:
                        ins.then_inc(esem[ename], 1)
                if ename == "sync":
                    for k, v in finals.items():
                        eng.wait_ge(dsem[k], v)

            @block.sync
            def _(e):
                run("sync", e)

            @block.scalar
            def _(e):
                run("act", e)

            @block.vector
            def _(e):
                run("dve", e)

            @block.gpsimd
            def _(e):
                run("pool", e)

            @block.tensor
            def _(e):
                run("pe", e)


ARENA_LO = 16512
ARENA_HI = 229376


class Prog:
    def __init__(self):
        self.nc = bass.Bass("TRN2", target_bir_lowering=False)
        self.S = Sched(self.nc)
        self.st = contextlib.ExitStack()
        self.top = ARENA_LO
        self.uid = 0

    def din(self, name, shape, dt):
        return self.nc.dram_tensor(name, list(shape), dt, kind="ExternalInput").ap()

    def dout(self, name, shape, dt):
        return self.nc.dram_tensor(name, list(shape), dt, kind="ExternalOutput").ap()

    def dscr(self, name, shape, dt):
        return self.nc.dram_tensor(name, list(shape), dt).ap()

    def sb(self, name, shape, dt):
        self.uid += 1
        esz = 2 if dt == BF16 else 4
        nbytes = esz
        for d in shape[1:]:
            nbytes *= d
        nbytes = (nbytes + 63) // 64 * 64
        assert self.top + nbytes <= ARENA_HI, ("SBUF arena overflow", name, self.top, nbytes)
        t = self.nc.alloc_sbuf_tensor_at("s%d_%s" % (self.uid, name), list(shape), dt, offset=self.top)
        self.top += nbytes
        return t, Buf(name)

    def mark(self):
        return self.top

    def release(self, mark):
        self.S.fence()
        self.top = mark

    def ps(self, name, shape=(128, 512), dt=F32):
        t = self.st.enter_context(self.nc.psum_tensor("p_" + name, list(shape), dt))
        return t, Buf(name)

    def ring(self, name, n, shape, dt):
        return Ring([self.sb("%s%d" % (name, i), shape, dt) for i in range(n)])

    def finish(self):
        self.S.emit()
        self.st.close()
        return self.nc


class Ring:
    def __init__(self, items):
        self.items = items
        self.i = 0

    def next(self):
        it = self.items[self.i % len(self.items)]
        self.i += 1
        return it


def emit_rstd(P, ss_ps, b_ss, n, inv_count, rs, b_rs):
    S = P.S
    S.op("act", lambda e: e.activation(out=rs[:, :n], in_=ss_ps[:, :n], func=AF.Sqrt, bias=P.epsc[:, 0:1], scale=inv_count),
         reads=[b_ss, P.b_eps], writes=[b_rs])
    S.op("dve", lambda e: e.reciprocal(out=rs[:, :n], in_=rs[:, :n]), reads=[b_rs], writes=[b_rs])


class XSrc:
    def __init__(self, P, xT_d, resident):
        self.P, self.xT_d, self.resident = P, xT_d, resident
        self.got = {}
        if resident:
            self.xT, _ = P.sb("xT", [128, KC, T], F32)
            self.bufs = [Buf("x%d" % i) for i in range(len(CHUNKS))]
        else:
            self.ring = P.ring("xr", 2, [128, KC, 512], F32)

    def get(self, ci):
        if ci in self.got:
            return self.got[ci]
        t0, n = CHUNKS[ci]
        if self.resident:
            tile, c0, buf = self.xT, t0, self.bufs[ci]
        else:
            tile, buf = self.ring.next()
            c0 = 0
        self.P.S.dma("sync", lambda e: e.dma_start(out=tile[:, :, c0:c0 + n], in_=self.xT_d[:, :, t0:t0 + n]), key=buf, writes=[buf])
        self.got[ci] = (tile, c0, buf)
        return self.got[ci]


def emit_norm_mod(P, xs, hT, b_h, j_sh, j_sc, ps_ring, sq_ring, rs_ring, tmp_ring, skip0=False):
    S = P.S
    modT, b_mod, scp = P.modT, P.b_mod, P.scp
    xs.get(1 if skip0 else 0)
    for ci, (t0, n) in enumerate(CHUNKS):
        if skip0 and ci == 0:
            continue
        t = 0 if ci == 0 else 1
        if ci + 1 < len(CHUNKS):
            xs.get(ci + 1)
        xt, c0, b_xc = xs.get(ci)
        ss, b_ss = ps_ring.next()
        for k in range(KC):
            sq, b_sq = sq_ring.next()
            S.op("act", lambda e: e.activation(out=sq[:, :n], in_=xt[:, k, c0:c0 + n], func=AF.Square), reads=[b_xc], writes=[b_sq])
            S.op("pe", lambda e: e.matmul(ss[:, :n], lhsT=P.ones[:], rhs=sq[:, :n], start=(k == 0), stop=(k == KC - 1)),
                 reads=[b_sq, P.b_ones], writes=[b_ss])
        rs, b_rs = rs_ring.next()
        emit_rstd(P, ss, b_ss, n, 1.0 / D, rs, b_rs)
        for k in range(KC):
            tmp, b_tmp = tmp_ring.next()
            S.op("dve", lambda e: e.scalar_tensor_tensor(out=tmp[:, :n], in0=xt[:, k, c0:c0 + n], scalar=scp[:, j_sc + k, t:t + 1], in1=rs[:, :n],
                                                         op0=ALU.mult, op1=ALU.mult), reads=[b_xc, b_rs, b_mod], writes=[b_tmp])
            S.op("act", lambda e: e.activation(out=hT[:, k, t0:t0 + n], in_=tmp[:, :n], func=AF.Identity, bias=modT[:, j_sh + k, t:t + 1], scale=1.0),
                 reads=[b_tmp, b_mod], writes=[b_h[ci]])


def load_w_bf16(P, name, w_d, ncols, col_split=1):
    S = P.S
    w, b_w = P.sb(name, [128, KC, ncols], BF16)
    wv = w_d.rearrange("(k p) n -> p k n", p=128)
    cs = ncols // col_split
    for k in range(KC):
        for c in range(col_split):
            S.dma("pool", lambda e: e.dma_start(out=w[:, k, c * cs:(c + 1) * cs], in_=wv[:, k, c * cs:(c + 1) * cs]), key=b_w, writes=[b_w])
    return w, b_w


def phase_mod(P, l):
    S = P.S
    mk = P.mark()
    D_ = P.D
    nj = 48
    wring = P.ring("wada", 2, [128, KC, 512], BF16)
    bT, b_bT = P.sb("bT", [128, nj], F32)
    S.dma("sync", lambda e: e.dma_start(out=bT[:], in_=D_["b_ada"][l]), key=b_bT, writes=[b_bT])
    pm, b_pm = P.bank[7]
    wv = D_["w_ada"][l].rearrange("(k p) n -> p k n", p=128)
    for blk in range(12):
        wt, b_wt = wring.next()
        for k in range(KC):
            S.dma("pool", lambda e: e.dma_start(out=wt[:, k, :], in_=wv[:, k, blk * 512:(blk + 1) * 512]), key=b_wt, writes=[b_wt])
        for jj in range(4):
            j = blk * 4 + jj
            for k in range(KC):
                S.op("pe", lambda e: e.matmul(pm[:, 2 * j:2 * j + 2], lhsT=wt[:, k, jj * 128:(jj + 1) * 128], rhs=P.cvb[:, k, :],
                                              start=(k == 0), stop=(k == KC - 1)), reads=[b_wt, P.b_cvb], writes=[b_pm])
    pmv = pm[:, 0:2 * nj].rearrange("p (j t) -> p j t", t=2)
    for t in range(2):
        S.op("dve", lambda e: e.tensor_tensor(out=P.modT[:, :, t], in0=pmv[:, :, t], in1=bT[:], op=ALU.add), reads=[b_pm, b_bT], writes=[P.b_mod])
    S.op("dve", lambda e: e.tensor_scalar(out=P.scp[:], in0=P.modT[:], scalar1=1.0, scalar2=None, op0=ALU.add), reads=[P.b_mod], writes=[P.b_mod])
    P.release(mk)


def phase_A(P, l, q, Xin):
    S = P.S
    D_ = P.D
    mk = P.mark()
    even = (l % 2 == 0)
    i = l // 2
    nq = 6 if even else 8
    nk = 2 if even else 8
    vw = 256 if even else 1024
    win_cols = 1536 if even else 3072
    win_d = (D_["w_in_even"] if even else D_["w_in_odd"])[i]
    cosT_d = (D_["ropecos128"] if even else D_["ropecos64"])[q]
    sinT_d = (D_["ropesin128"] if even else D_["ropesin64"])[q]
    PT = P.PT128 if even else P.PT64
    b_PT = P.b_const
    psA = Ring([P.bank[0], P.bank[1], P.bank[2]])
    psB = Ring([P.bank[3], P.bank[4]])

    xs = XSrc(P, Xin[q], resident=False)
    hT, _ = P.sb("hT", [128, KC, T], BF16)
    b_h = [Buf("h%d" % c) for c in range(len(CHUNKS))]
    sq_ring = P.ring("sq", 2, [128, 512], BF16)
    rs_ring = P.ring("rs", 2, [128, 512], F32)
    tmp_ring = P.ring("tmp", 2, [128, 512], F32)
    skip0 = (q != 0)
    emit_norm_mod(P, xs, hT, b_h, 0, KC, psB, sq_ring, rs_ring, tmp_ring, skip0=skip0)

    w, b_w = load_w_bf16(P, "win", win_d, win_cols, col_split=(1 if even else 2))
    cosT, b_cos = P.sb("cosT", [128, T], F32)
    sinT, b_sin = P.sb("sinT", [128, T], F32)
    S.dma("sync", lambda e: e.dma_start(out=cosT[:], in_=cosT_d), key=b_cos, writes=[b_cos])
    S.dma("sync", lambda e: e.dma_start(out=sinT[:], in_=sinT_d), key=b_sin, writes=[b_sin])
    if even:
        gains, b_g = P.sb("gains", [128, 4], F32)
        S.dma("sync", lambda e: e.dma_start(out=gains[:], in_=D_["gains"][i]), key=b_g, writes=[b_g])

    qn_ring = P.ring("qn", 2, [128, 512], F32)
    qnb_ring = P.ring("qnb", 2, [128, 512], BF16)
    t1_ring = P.ring("t1", 2, [128, 512], F32)
    t2_ring = P.ring("t2", 2, [128, 512], F32)
    stage_ring = P.ring("stg", 2, [128, T], BF16)
    Qs, Ks = P.Qs, P.Ks

    for hc in range(nq + nk):
        is_q = hc < nq
        c0 = hc * 128
        stg, b_stg = stage_ring.next()
        for ci, (t0, n) in enumerate(CHUNKS):
            if skip0 and ci == 0:
                continue
            pq, b_pq = psA.next()
            for k in range(KC):
                S.op("pe", lambda e: e.matmul(pq[:, :n], lhsT=w[:, k, c0:c0 + 128], rhs=hT[:, k, t0:t0 + n], start=(k == 0), stop=(k == KC - 1)),
                     reads=[b_w, b_h[ci]], writes=[b_pq])
            qn, b_qn = qn_ring.next()
            if even:
                sq, b_sq = sq_ring.next()
                S.op("act", lambda e: e.activation(out=sq[:, :n], in_=pq[:, :n], func=AF.Square), reads=[b_pq], writes=[b_sq])
                ss, b_ss = psB.next()
                S.op("pe", lambda e: e.matmul(ss[:, :n], lhsT=P.ones[:], rhs=sq[:, :n], start=True, stop=True), reads=[b_sq, P.b_ones], writes=[b_ss])
                rs, b_rs = rs_ring.next()
                emit_rstd(P, ss, b_ss, n, 1.0 / 128, rs, b_rs)
                gcol = 0 if is_q else 1
                S.op("dve", lambda e: e.scalar_tensor_tensor(out=qn[:, :n], in0=pq[:, :n], scalar=gains[:, gcol:gcol + 1], in1=rs[:, :n],
                                                             op0=ALU.mult, op1=ALU.mult), reads=[b_pq, b_rs, b_g], writes=[b_qn])
            else:
                S.op("act", lambda e: e.copy(out=qn[:, :n], in_=pq[:, :n]), reads=[b_pq], writes=[b_qn])
            qnb, b_qnb = qnb_ring.next()
            S.op("act", lambda e: e.copy(out=qnb[:, :n], in_=qn[:, :n]), reads=[b_qn], writes=[b_qnb])
            pr, b_pr = psB.next()
            S.op("pe", lambda e: e.matmul(pr[:, :n], lhsT=PT[:], rhs=qnb[:, :n], start=True, stop=True), reads=[b_qnb, b_PT], writes=[b_pr])
            t1, b_t1 = t1_ring.next()
            S.op("pool", lambda e: e.tensor_tensor(out=t1[:, :n], in0=qn[:, :n], in1=cosT[:, t0:t0 + n], op=ALU.mult), reads=[b_qn, b_cos], writes=[b_t1])
            t2, b_t2 = t2_ring.next()
            S.op("dve", lambda e: e.tensor_tensor(out=t2[:, :n], in0=pr[:, :n], in1=sinT[:, t0:t0 + n], op=ALU.mult), reads=[b_pr, b_sin], writes=[b_t2])
            S.op("dve", lambda e: e.tensor_tensor(out=stg[:, t0:t0 + n], in0=t1[:, :n], in1=t2[:, :n], op=ALU.add), reads=[b_t1, b_t2], writes=[b_stg])
        if is_q:
            c_lo = CTX if skip0 else 0
            S.dma("sync", lambda e: e.dma_start(out=Qs[q, hc, :, c_lo:T], in_=stg[:, c_lo:T]), key=b_stg, reads=[b_stg])
        else:
            kh = hc - nq
            if q == 0:
                S.dma("sync", lambda e: e.dma_start(out=Ks[kh, :, 0:CTX], in_=stg[:, 0:CTX]), key=b_stg, reads=[b_stg])
            S.dma("sync", lambda e: e.dma_start(out=Ks[kh, :, CTX + q * TL:CTX + (q + 1) * TL], in_=stg[:, CTX:T]), key=b_stg, reads=[b_stg])

    vc0 = (nq + nk) * 128
    vst_ring = P.ring("vst", 2, [128, vw], BF16)
    for tt in range(NT):
        if tt < 2 and q != 0:
            continue
        gt = tt if tt < 2 else 2 + q * 16 + (tt - 2)
        ci = 0 if tt < 2 else 1 + (tt - 2) // 4
        vst, b_vst = vst_ring.next()
        cw = min(vw, 512)
        for cb in range(max(1, vw // 512)):
            pv, b_pv = psA.next()
            for k in range(KC):
                S.op("pe", lambda e: e.matmul(pv[:, :cw], lhsT=hT[:, k, tt * 128:(tt + 1) * 128], rhs=w[:, k, vc0 + cb * cw:vc0 + (cb + 1) * cw],
                                              start=(k == 0), stop=(k == KC - 1)), reads=[b_w, b_h[ci]], writes=[b_pv])
            S.op("act", lambda e: e.copy(out=vst[:, cb * cw:(cb + 1) * cw], in_=pv[:, :cw]), reads=[b_pv], writes=[b_vst])
        if even:
            S.dma("sync", lambda e: e.dma_start(out=P.Vs[:, gt, :], in_=vst[:]), key=b_vst, reads=[b_vst])
        else:
            S.dma("sync", lambda e: e.dma_start(out=P.Vo[:, :, gt, :].rearrange("h p d -> p h d"), in_=vst[:].rearrange("p (h d) -> p h d", h=8)),
                  key=b_vst, reads=[b_vst])

    if even:
        fc0 = 1280
        gcst, b_gcst = P.sb("gcst", [128, NT, 256], BF16)
        gsst, b_gsst = P.sb("gsst", [128, NT, 256], BF16)
        gT_ring = P.ring("gT", 2, [128, 512], BF16)
        for jc in range(2):
            for ci, (t0, n) in enumerate(CHUNKS):
                if ci == 0 and q != 0:
                    continue
                pf, b_pf = psA.next()
                for k in range(KC):
                    S.op("pe", lambda e: e.matmul(pf[:, :n], lhsT=w[:, k, fc0 + jc * 128:fc0 + (jc + 1) * 128], rhs=hT[:, k, t0:t0 + n],
                                                  start=(k == 0), stop=(k == KC - 1)), reads=[b_w, b_h[ci]], writes=[b_pf])
                sq, b_sq = sq_ring.next()
                S.op("act", lambda e: e.activation(out=sq[:, :n], in_=pf[:, :n], func=AF.Square), reads=[b_pf], writes=[b_sq])
                ss, b_ss = psB.next()
                S.op("pe", lambda e: e.matmul(ss[:, :n], lhsT=P.bdo[:], rhs=sq[:, :n], start=True, stop=True), reads=[b_sq, P.b_const], writes=[b_ss])
                rs, b_rs = rs_ring.next()
                emit_rstd(P, ss, b_ss, n, 1.0 / 64, rs, b_rs)
                gT, b_gT = gT_ring.next()
                S.op("dve", lambda e: e.scalar_tensor_tensor(out=gT[:, :n], in0=pf[:, :n], scalar=gains[:, 2 + jc:3 + jc], in1=rs[:, :n],
                                                             op0=ALU.mult, op1=ALU.mult), reads=[b_pf, b_rs, b_g], writes=[b_gT])
                for ti in range(n // 128):
                    tt = t0 // 128 + ti
                    pg, b_pg = psB.next()
                    S.op("pe", lambda e: e.matmul(pg[:, 0:128], lhsT=gT[:, ti * 128:(ti + 1) * 128], rhs=P.bdc[:], start=True, stop=True),
                         reads=[b_gT, P.b_const], writes=[b_pg])
                    S.op("pe", lambda e: e.matmul(pg[:, 128:256], lhsT=gT[:, ti * 128:(ti + 1) * 128], rhs=P.bds[:], start=True, stop=True),
                         reads=[b_gT, P.b_const], writes=[b_pg])
                    S.op("act", lambda e: e.copy(out=gcst[:, tt, jc * 128:(jc + 1) * 128], in_=pg[:, 0:128]), reads=[b_pg], writes=[b_gcst])
                    S.op("act", lambda e: e.copy(out=gsst[:, tt, jc * 128:(jc + 1) * 128], in_=pg[:, 128:256]), reads=[b_pg], writes=[b_gsst])
        for st_, sc_, cc_, bb in ((gcst, P.GCs, P.GCc, b_gcst), (gsst, P.GSs, P.GSc, b_gsst)):
            if q == 0:
                S.dma("sync", lambda e: e.dma_start(out=cc_, in_=st_[:, 0:2, :]), key=bb, reads=[bb])
            S.dma("sync", lambda e: e.dma_start(out=sc_[:, q * 16:(q + 1) * 16, :], in_=st_[:, 2:NT, :]), key=bb, reads=[bb])
    P.release(mk)


class AttnRes:
    pass


def attn_setup(P):
    R = AttnRes()
    R.S_ring = Ring([P.bank[0], P.bank[1], P.bank[2]])
    R.O_ring = Ring([P.bank[3], P.bank[4]])
    R.Z_ring = Ring([P.bank[5], P.bank[6]])
    R.pt_ring = P.ring("pt", 6, [128, 512], BF16)
    R.rz_ring = P.ring("rz", 2, [128, 512], F32)
    R.accP_ring = P.ring("accP", 2, [128, 512], F32)
    R.accD_ring = P.ring("accD", 2, [128, 512], F32)
    return R


def attn_unit(P, R, q_ap, b_q, ktiles, vtiles, n, scale, bias_ap, out_ap, b_out):
    S = P.S
    po, b_po = R.O_ring.next()
    pz, b_pz = R.Z_ring.next()
    accs = [R.accP_ring.next(), R.accD_ring.next()]
    acc_eng = ["dve", "dve"]
    nk = len(ktiles)
    assert nk >= 2
    LA = 2
    pts = {}
    for i in range(nk + LA):
        if i < nk:
            kap, b_k = ktiles[i]
            ps, b_ps = R.S_ring.next()
            S.op("pe", lambda e: e.matmul(ps[:, :n], lhsT=kap, rhs=q_ap, start=True, stop=True), reads=[b_k, b_q], writes=[b_ps])
            pt, b_pt = R.pt_ring.next()
            S.op("act", lambda e: e.activation(out=pt[:, :n], in_=ps[:, :n], func=AF.Exp, bias=bias_ap, scale=scale), reads=[b_ps, P.b_const], writes=[b_pt])
            pts[i] = (pt, b_pt)
        j = i - LA
        if j >= 0:
            vap, b_v = vtiles[j]
            pt, b_pt = pts.pop(j)
            S.op("pe", lambda e: e.matmul(po[:, :n], lhsT=vap, rhs=pt[:, :n], start=(j == 0), stop=(j == nk - 1)), reads=[b_v, b_pt], writes=[b_po])
            acc, b_acc = accs[j % 2]
            if j < 2:
                S.op(acc_eng[j % 2], lambda e: e.tensor_copy(out=acc[:, :n], in_=pt[:, :n]), reads=[b_pt], writes=[b_acc])
            else:
                S.op(acc_eng[j % 2], lambda e: e.tensor_tensor(out=acc[:, :n], in0=acc[:, :n], in1=pt[:, :n], op=ALU.add), reads=[b_pt, b_acc], writes=[b_acc])
    S.op("dve", lambda e: e.tensor_tensor(out=accs[1][0][:, :n], in0=accs[1][0][:, :n], in1=accs[0][0][:, :n], op=ALU.add),
         reads=[accs[0][1], accs[1][1]], writes=[accs[1][1]])
    S.op("pe", lambda e: e.matmul(pz[:, :n], lhsT=P.ones32[:], rhs=accs[1][0][:, :n], start=True, stop=True), reads=[P.b_ones, accs[1][1]], writes=[b_pz])
    rz, b_rz = R.rz_ring.next()
    S.op("dve", lambda e: e.reciprocal(out=rz[:, :n], in_=pz[:, :n]), reads=[b_pz], writes=[b_rz])
    S.op("dve", lambda e: e.tensor_tensor(out=out_ap, in0=po[:, :n], in1=rz[:, :n], op=ALU.mult), reads=[b_po, b_rz], writes=[b_out])


def emit_outproj(P, mixT, b_mix, wout_d, xin_d, xout_d, j_g, ps_ring, skip0=False):
    S = P.S
    wo, b_wo = load_w_bf16(P, "wout", wout_d, D)
    xin_ring = P.ring("xin", 3, [128, 512], F32)
    xout_ring = P.ring("xout", 3, [128, 512], F32)
    for ci, (t0, n) in enumerate(CHUNKS):
        if skip0 and ci == 0:
            continue
        t = 0 if ci == 0 else 1
        for m in range(KC):
            py, b_py = ps_ring.next()
            for j in range(KC):
                S.op("pe", lambda e: e.matmul(py[:, :n], lhsT=wo[:, j, m * 128:(m + 1) * 128], rhs=mixT[:, j, t0:t0 + n], start=(j == 0), stop=(j == KC - 1)),
                     reads=[b_wo, b_mix[ci]], writes=[b_py])
            xi, b_xi = xin_ring.next()
            S.dma("sync", lambda e: e.dma_start(out=xi[:, :n], in_=xin_d[:, m, t0:t0 + n]), key=b_xi, writes=[b_xi])
            xo, b_xo = xout_ring.next()
            S.op("dve", lambda e: e.scalar_tensor_tensor(out=xo[:, :n], in0=py[:, :n], scalar=P.modT[:, j_g + m, t:t + 1], in1=xi[:, :n],
                                                         op0=ALU.mult, op1=ALU.add), reads=[b_py, b_xi, P.b_mod], writes=[b_xo])
            S.dma("act", lambda e: e.dma_start(out=xout_d[:, m, t0:t0 + n], in_=xo[:, :n]), key=b_xo, reads=[b_xo])


def phase_B_even(P, l, q, Xin, Xout):
    S = P.S
    D_ = P.D
    mk = P.mark()
    i = l // 2
    R = attn_setup(P)
    mixT, _ = P.sb("mixT", [128, KC, T], BF16)
    b_mix = [Buf("mix%d" % c) for c in range(len(CHUNKS))]

    skip0 = (q != 0)
    gcC, b_gcC = P.sb("gcC", [128, 2, 256], BF16)
    gsC, b_gsC = P.sb("gsC", [128, 2, 256], BF16)
    cosC, b_cosC = P.sb("cosC", [128, 2, 256], BF16)
    nsinC, b_nsinC = P.sb("nsinC", [128, 2, 256], BF16)
    for tt, dd, bb in ((gcC, P.GCc, b_gcC), (gsC, P.GSc, b_gsC), (cosC, D_["cosC"], b_cosC), (nsinC, D_["nsinC"], b_nsinC)):
        S.dma("sync", lambda e: e.dma_start(out=tt[:], in_=dd), key=bb, writes=[bb])
    accs = [R.O_ring.next(), R.Z_ring.next()]
    for jc in range(0 if not skip0 else 2, 2):
        acc, b_acc = accs[jc]
        ops = []
        for nt in range(2):
            ops.append((gcC, b_gcC, cosC, b_cosC, nt))
            ops.append((gsC, b_gsC, nsinC, b_nsinC, nt))
        for ii, (g, b_g, tb, b_tb, nt) in enumerate(ops):
            S.op("pe", lambda e: e.matmul(acc[:, :256], lhsT=g[:, nt, jc * 128:(jc + 1) * 128], rhs=tb[:, nt, :], start=(ii == 0), stop=(ii == 3)),
                 reads=[b_g, b_tb], writes=[b_acc])
        S.op("act", lambda e: e.copy(out=mixT[:, 6 + jc, 0:256], in_=acc[:, :256]), reads=[b_acc], writes=[b_mix[0]])
    NG = 4
    cos_ring = P.ring("cosp", 2, [128, NG, 512], BF16)
    sin_ring = P.ring("sinp", 2, [128, NG, 512], BF16)
    gc_ring = P.ring("gcp", 2, [128, NG, 256], BF16)
    gs_ring = P.ring("gsp", 2, [128, NG, 256], BF16)
    cosL_d, nsinL_d = D_["cosL"][q], D_["nsinL"][q]
    for kc in range(4):
        ci = kc + 1
        t0, n = CHUNKS[ci]
        accs = [R.O_ring.next(), R.Z_ring.next()]
        for ng in range(64 // NG):
            cp, b_cp = cos_ring.next()
            sp, b_sp = sin_ring.next()
            gp, b_gp = gc_ring.next()
            hp, b_hp = gs_ring.next()
            n0 = ng * NG
            S.dma("sync", lambda e: e.dma_start(out=cp[:], in_=cosL_d[kc, :, n0:n0 + NG, :]), key=b_cp, writes=[b_cp])
            S.dma("sync", lambda e: e.dma_start(out=sp[:], in_=nsinL_d[kc, :, n0:n0 + NG, :]), key=b_sp, writes=[b_sp])
            S.dma("sync", lambda e: e.dma_start(out=gp[:], in_=P.GCs[:, n0:n0 + NG, :]), key=b_gp, writes=[b_gp])
            S.dma("sync", lambda e: e.dma_start(out=hp[:], in_=P.GSs[:, n0:n0 + NG, :]), key=b_hp, writes=[b_hp])
            for nt in range(NG):
                first = (ng == 0 and nt == 0)
                last = (ng == 64 // NG - 1 and nt == NG - 1)
                for jc in range(2):
                    acc, b_acc = accs[jc]
                    S.op("pe", lambda e: e.matmul(acc[:, :], lhsT=gp[:, nt, jc * 128:(jc + 1) * 128], rhs=cp[:, nt, :], start=first, stop=False),
                         reads=[b_gp, b_cp], writes=[b_acc])
                    S.op("pe", lambda e: e.matmul(acc[:, :], lhsT=hp[:, nt, jc * 128:(jc + 1) * 128], rhs=sp[:, nt, :], start=False, stop=last),
                         reads=[b_hp, b_sp], writes=[b_acc])
        for jc in range(2):
            acc, b_acc = accs[jc]
            S.op("act", lambda e: e.copy(out=mixT[:, 6 + jc, t0:t0 + 512], in_=acc[:, :]), reads=[b_acc], writes=[b_mix[ci]])

    Ksb, b_K = P.sb("Ksb", [128, 2, NKEY], BF16)
    Vsb, b_V = P.sb("Vsb", [128, NKT, 256], BF16)
    for h in range(2):
        S.dma("sync", lambda e: e.dma_start(out=Ksb[:, h, :], in_=P.Ks[h]), key=b_K, writes=[b_K])
    for hh in range(2):
        S.dma("sync", lambda e: e.dma_start(out=Vsb[:, hh * 33:(hh + 1) * 33, :], in_=P.Vs[:, hh * 33:(hh + 1) * 33, :]), key=b_V, writes=[b_V])
    q_ring = P.ring("qsb", 2, [128, T], BF16)
    scale = 128 ** -0.5
    for h in range(6):
        kvh = h // 3
        qs, b_qs = q_ring.next()
        S.dma("sync", lambda e: e.dma_start(out=qs[:], in_=P.Qs[q, h]), key=b_qs, writes=[b_qs])
        for ci, (t0, n) in enumerate(CHUNKS):
            if skip0 and ci == 0:
                continue
            nkt = 2 if ci == 0 else NKT
            ktiles = [(Ksb[:, kvh, j * 128:(j + 1) * 128], b_K) for j in range(nkt)]
            vtiles = [(Vsb[:, j, kvh * 128:(kvh + 1) * 128], b_V) for j in range(nkt)]
            attn_unit(P, R, qs[:, t0:t0 + n], b_qs, ktiles, vtiles, n, scale, P.biasm8[:, 0:1], mixT[:, h, t0:t0 + n], b_mix[ci])

    emit_outproj(P, mixT, b_mix, D_["w_out_even"][i], Xin[q], Xout[q], 16, R.S_ring, skip0=skip0)
    P.release(mk)


def phase_lam(P, l):
    S = P.S
    D_ = P.D
    mk = P.mark()
    i = l // 2
    lam_init = 0.8 - 0.6 * math.exp(-0.3 * l)
    lamv, b_lamv = P.sb("lamv", [128, 4, 64], F32)
    S.dma("sync", lambda e: e.dma_start(out=lamv[:], in_=D_["lamv"][i]), key=b_lamv, writes=[b_lamv])
    lp, b_lp = P.sb("lp", [128, 2, 64], F32)
    S.op("dve", lambda e: e.tensor_tensor(out=lp[:, 0, :], in0=lamv[:, 0, :], in1=lamv[:, 1, :], op=ALU.mult), reads=[b_lamv], writes=[b_lp])
    S.op("dve", lambda e: e.tensor_tensor(out=lp[:, 1, :], in0=lamv[:, 2, :], in1=lamv[:, 3, :], op=ALU.mult), reads=[b_lamv], writes=[b_lp])
    ls, b_ls = P.sb("ls", [128, 2], F32)
    S.op("dve", lambda e: e.reduce_sum(out=ls[:], in_=lp[:], axis=AX.X), reads=[b_lp], writes=[b_ls])
    le, b_le = P.sb("le", [128, 2], F32)
    S.op("act", lambda e: e.activation(out=le[:], in_=ls[:], func=AF.Exp), reads=[b_ls], writes=[b_le])
    b_n = P.b_lam
    S.op("dve", lambda e: e.tensor_tensor(out=P.nlam[:], in0=le[:, 1:2], in1=le[:, 0:1], op=ALU.subtract), reads=[b_le], writes=[b_n])
    S.op("dve", lambda e: e.tensor_scalar(out=P.nlam[:], in0=P.nlam[:], scalar1=-lam_init, scalar2=None, op0=ALU.add), reads=[b_n], writes=[b_n])
    hgr, b_hgr = P.sb("hgr", [128, 1], F32)
    S.dma("sync", lambda e: e.dma_start(out=hgr[:], in_=D_["hgain"][i]), key=b_hgr, writes=[b_hgr])
    S.op("dve", lambda e: e.tensor_scalar(out=P.hg[:], in0=hgr[:], scalar1=(1.0 - lam_init), scalar2=None, op0=ALU.mult), reads=[b_hgr], writes=[b_n])
    P.release(mk)


def phase_B_odd(P, l, q, Xin, Xout):
    S = P.S
    D_ = P.D
    mk = P.mark()
    i = l // 2
    R = attn_setup(P)
    skip0 = (q != 0)
    nlam, hg, b_n = P.nlam, P.hg, P.b_lam
    mixT, _ = P.sb("mixT", [128, KC, T], BF16)
    b_mix = [Buf("mix%d" % c) for c in range(len(CHUNKS))]
    k_ring = P.ring("ksb", 2, [128, NKEY], BF16)
    v_ring = P.ring("vsb", 2, [128, NKT, 128], BF16)
    q0_ring = P.ring("q0p", 2, [128, T], BF16)
    q1_ring = P.ring("q1p", 2, [128, T], BF16)
    for (qt, b_qt) in q0_ring.items:
        S.op("pool", lambda e: e.memset(qt[64:128, :], 0.0), writes=[b_qt])
    for (qt, b_qt) in q1_ring.items:
        S.op("pool", lambda e: e.memset(qt[0:64, :], 0.0), writes=[b_qt])
    a_ring = P.ring("am", 4, [128, 512], F32)
    o_ring = P.ring("ocomb", 2, [128, 512], F32)
    sq_ring = P.ring("sq", 2, [128, 512], BF16)
    rs_ring = P.ring("rs", 2, [128, 512], F32)
    scale = 64 ** -0.5
    for h in range(8):
        ks, b_ks = k_ring.next()
        vs, b_vs = v_ring.next()
        q0, b_q0 = q0_ring.next()
        q1, b_q1 = q1_ring.next()
        S.dma("sync", lambda e: e.dma_start(out=ks[:], in_=P.Ks[h]), key=b_ks, writes=[b_ks])
        for hh in range(2):
            S.dma("sync", lambda e: e.dma_start(out=vs[:, hh * 33:(hh + 1) * 33, :], in_=P.Vo[h, :, hh * 33:(hh + 1) * 33, :]), key=b_vs, writes=[b_vs])
        S.dma("sync", lambda e: e.dma_start(out=q0[0:64, :], in_=P.Qs[q, h, 0:64, :]), key=b_q0, writes=[b_q0])
        S.dma("sync", lambda e: e.dma_start(out=q1[64:128, :], in_=P.Qs[q, h, 64:128, :]), key=b_q1, writes=[b_q1])
        qm = [(q0, b_q0), (q1, b_q1)]
        for ci, (t0, n) in enumerate(CHUNKS):
            if skip0 and ci == 0:
                continue
            nkt = 2 if ci == 0 else NKT
            am = []
            for m in range(2):
                ktiles = [(ks[:, j * 128:(j + 1) * 128], b_ks) for j in range(nkt)]
                vtiles = [(vs[:, j, :], b_vs) for j in range(nkt)]
                a, b_a = a_ring.next()
                attn_unit(P, R, qm[m][0][:, t0:t0 + n], qm[m][1], ktiles, vtiles, n, scale, P.bias0[:, 0:1], a[:, :n], b_a)
                am.append((a, b_a))
            oc, b_oc = o_ring.next()
            S.op("dve", lambda e: e.scalar_tensor_tensor(out=oc[:, :n], in0=am[1][0][:, :n], scalar=nlam[:, 0:1], in1=am[0][0][:, :n],
                                                         op0=ALU.mult, op1=ALU.add), reads=[am[0][1], am[1][1], b_n], writes=[b_oc])
            sq, b_sq = sq_ring.next()
            S.op("act", lambda e: e.activation(out=sq[:, :n], in_=oc[:, :n], func=AF.Square), reads=[b_oc], writes=[b_sq])
            ss, b_ss = R.S_ring.next()
            S.op("pe", lambda e: e.matmul(ss[:, :n], lhsT=P.ones[:], rhs=sq[:, :n], start=True, stop=True), reads=[b_sq, P.b_ones], writes=[b_ss])
            rs, b_rs = rs_ring.next()
            emit_rstd(P, ss, b_ss, n, 1.0 / 128, rs, b_rs)
            S.op("dve", lambda e: e.scalar_tensor_tensor(out=mixT[:, h, t0:t0 + n], in0=oc[:, :n], scalar=hg[:, 0:1], in1=rs[:, :n],
                                                         op0=ALU.mult, op1=ALU.mult), reads=[b_oc, b_rs, b_n], writes=[b_mix[ci]])

    emit_outproj(P, mixT, b_mix, D_["w_out_odd"][i], Xin[q], Xout[q], 16, R.S_ring, skip0=skip0)
    P.release(mk)


def phase_C(P, l, q, Xin, Xout, last):
    S = P.S
    D_ = P.D
    mk = P.mark()
    psA = Ring([P.bank[3], P.bank[4], P.bank[5], P.bank[6]])
    psD = Ring([P.bank[0], P.bank[1]])
    psG = P.bank[2]
    modT, b_mod = P.modT, P.b_mod
    skip0 = (q != 0) or last
    xs = XSrc(P, Xin[q], resident=True)
    xT, b_x = xs.xT, xs.bufs
    hT, _ = P.sb("hT", [128, KC, T], BF16)
    b_h = [Buf("h%d" % c) for c in range(len(CHUNKS))]
    sq_ring = P.ring("sq", 2, [128, 512], BF16)
    rs_ring = P.ring("rs", 2, [128, 512], F32)
    tmp_ring = P.ring("tmp", 2, [128, 512], F32)
    emit_norm_mod(P, xs, hT, b_h, 24, 32, psD, sq_ring, rs_ring, tmp_ring, skip0=skip0)
    gT, b_gT = P.sb("gT", [16, T], BF16)
    mk2 = P.mark()

    wr, b_wr = P.sb("wr", [128, KC, 20], BF16)
    wrv = D_["wr"][l].rearrange("(k p) n -> p k n", p=128)
    for k in range(KC):
        S.dma("pool", lambda e: e.dma_start(out=wr[:, k, :], in_=wrv[:, k, :]), key=b_wr, writes=[b_wr])
    br, b_br = P.sb("br", [128, 20], F32)
    S.dma("sync", lambda e: e.dma_start(out=br[:], in_=D_["br"][l]), key=b_br, writes=[b_br])
    L, b_L = P.sb("L", [128, NT, 20], F32)
    for tt in range(NT):
        if skip0 and tt < 2:
            continue
        ci = 0 if tt < 2 else 1 + (tt - 2) // 4
        pl, b_pl = psD.next()
        for k in range(KC):
            S.op("pe", lambda e: e.matmul(pl[:, 0:20], lhsT=hT[:, k, tt * 128:(tt + 1) * 128], rhs=wr[:, k, :], start=(k == 0), stop=(k == KC - 1)),
                 reads=[b_wr, b_h[ci]], writes=[b_pl])
        S.op("dve", lambda e: e.tensor_tensor(out=L[:, tt, :], in0=pl[:, 0:20], in1=br[:], op=ALU.add), reads=[b_pl, b_br], writes=[b_L])

    cnt = [0]

    def tmp():
        cnt[0] += 1
        return P.sb("g%d" % cnt[0], [128, NT], F32)

    def tt_(out, a, b, op, rd, wr_):
        S.op("dve", lambda e: e.tensor_tensor(out=out, in0=a, in1=b, op=op), reads=rd, writes=wr_)

    gl = [L[:, :, g] for g in range(4)]
    m01, b_m01 = tmp()
    m23, b_m23 = tmp()
    gmax, b_gmax = tmp()
    tt_(m01[:], gl[0], gl[1], ALU.max, [b_L], [b_m01])
    tt_(m23[:], gl[2], gl[3], ALU.max, [b_L], [b_m23])
    tt_(gmax[:], m01[:], m23[:], ALU.max, [b_m01, b_m23], [b_gmax])
    masks = []
    sumexp, b_se = tmp()
    for g in range(4):
        dg, b_dg = tmp()
        tt_(dg[:], gl[g], gmax[:], ALU.subtract, [b_L, b_gmax], [b_dg])
        eg, b_eg = tmp()
        S.op("act", lambda e: e.activation(out=eg[:], in_=dg[:], func=AF.Exp), reads=[b_dg], writes=[b_eg])
        if g == 0:
            S.op("dve", lambda e: e.tensor_copy(out=sumexp[:], in_=eg[:]), reads=[b_eg], writes=[b_se])
        else:
            tt_(sumexp[:], sumexp[:], eg[:], ALU.add, [b_se, b_eg], [b_se])
        mk_, b_mk = tmp()
        tt_(mk_[:], gl[g], gmax[:], ALU.is_equal, [b_L, b_gmax], [b_mk])
        masks.append((mk_, b_mk))
    ggate, b_gg = tmp()
    S.op("dve", lambda e: e.reciprocal(out=ggate[:], in_=sumexp[:]), reads=[b_se], writes=[b_gg])
    esel = []
    for j in range(4):
        es, b_es = tmp()
        pr_, b_pr_ = tmp()
        for g in range(4):
            mk_, b_mk = masks[g]
            if g == 0:
                tt_(es[:], L[:, :, 4 + g * 4 + j], mk_[:], ALU.mult, [b_L, b_mk], [b_es])
            else:
                tt_(pr_[:], L[:, :, 4 + g * 4 + j], mk_[:], ALU.mult, [b_L, b_mk], [b_pr_])
                tt_(es[:], es[:], pr_[:], ALU.add, [b_es, b_pr_], [b_es])
        esel.append((es, b_es))

    def max4(items):
        a, b_a = tmp()
        b, b_b = tmp()
        c, b_c = tmp()
        tt_(a[:], items[0][0][:], items[1][0][:], ALU.max, [items[0][1], items[1][1]], [b_a])
        tt_(b[:], items[2][0][:], items[3][0][:], ALU.max, [items[2][1], items[3][1]], [b_b])
        tt_(c[:], a[:], b[:], ALU.max, [b_a, b_b], [b_c])
        return c, b_c

    e1, b_e1 = max4(esel)
    m1 = []
    esel2 = []
    for j in range(4):
        mk_, b_mk = tmp()
        tt_(mk_[:], esel[j][0][:], e1[:], ALU.is_equal, [esel[j][1], b_e1], [b_mk])
        m1.append((mk_, b_mk))
        e2_, b_e2_ = tmp()
        S.op("dve", lambda e: e.scalar_tensor_tensor(out=e2_[:], in0=mk_[:], scalar=-1e30, in1=esel[j][0][:], op0=ALU.mult, op1=ALU.add),
             reads=[b_mk, esel[j][1]], writes=[b_e2_])
        esel2.append((e2_, b_e2_))
    e2, b_e2 = max4(esel2)
    m2 = []
    for j in range(4):
        mk_, b_mk = tmp()
        tt_(mk_[:], esel2[j][0][:], e2[:], ALU.is_equal, [esel2[j][1], b_e2], [b_mk])
        m2.append((mk_, b_mk))
    dd, b_dd = tmp()
    tt_(dd[:], e2[:], e1[:], ALU.subtract, [b_e2, b_e1], [b_dd])
    rr, b_rr = tmp()
    S.op("act", lambda e: e.activation(out=rr[:], in_=dd[:], func=AF.Exp), reads=[b_dd], writes=[b_rr])
    den, b_den = tmp()
    S.op("dve", lambda e: e.tensor_scalar(out=den[:], in0=rr[:], scalar1=1.0, scalar2=None, op0=ALU.add), reads=[b_rr], writes=[b_den])
    S.op("dve", lambda e: e.reciprocal(out=den[:], in_=den[:]), reads=[b_den], writes=[b_den])
    w1, b_w1 = tmp()
    tt_(w1[:], ggate[:], den[:], ALU.mult, [b_gg, b_den], [b_w1])
    w2, b_w2 = tmp()
    tt_(w2[:], w1[:], rr[:], ALU.mult, [b_w1, b_rr], [b_w2])
    G, b_G = P.sb("G", [128, NT, 16], F32)
    for j in range(4):
        ew, b_ew = tmp()
        e2w, b_e2w = tmp()
        tt_(ew[:], m1[j][0][:], w1[:], ALU.mult, [m1[j][1], b_w1], [b_ew])
        tt_(e2w[:], m2[j][0][:], w2[:], ALU.mult, [m2[j][1], b_w2], [b_e2w])
        tt_(ew[:], ew[:], e2w[:], ALU.add, [b_ew, b_e2w], [b_ew])
        for g in range(4):
            tt_(G[:, :, g * 4 + j], masks[g][0][:], ew[:], ALU.mult, [masks[g][1], b_ew], [b_G])

    for tt in range(NT):
        if skip0 and tt < 2:
            continue
        pt_, b_pt_ = psD.next()
        S.op("pe", lambda e: e.matmul(pt_[0:16, 0:128], lhsT=G[:, tt, :], rhs=P.ident[:], start=True, stop=True), reads=[b_G, P.b_const], writes=[b_pt_])
        S.op("act", lambda e: e.copy(out=gT[:, tt * 128:(tt + 1) * 128], in_=pt_[0:16, 0:128]), reads=[b_pt_], writes=[b_gT])

    P.release(mk2)
    wg_ring = P.ring("wg", 2, [128, KC, 512], BF16)
    wu_ring = P.ring("wu", 2, [128, KC, 512], BF16)
    wd_ring = P.ring("wd", 2, [128, 4, D], BF16)
    sil_ring = P.ring("sil", 2, [128, 512], F32)
    us_ring = P.ring("us", 2, [128, 512], F32)
    a_ring = P.ring("aT", 8, [128, 512], BF16)
    wts = {}

    def load_expert(ex):
        wg, b_wg = wg_ring.next()
        wu, b_wu = wu_ring.next()
        wd, b_wd = wd_ring.next()
        wgv = D_["w_gate"][l, ex].rearrange("(k p) n -> p k n", p=128)
        wuv = D_["w_up"][l, ex].rearrange("(k p) n -> p k n", p=128)
        wdv = D_["w_down"][l, ex].rearrange("(k p) n -> p k n", p=128)
        for k in range(KC):
            S.dma("pool", lambda e: e.dma_start(out=wg[:, k, :], in_=wgv[:, k, :]), key=b_wg, writes=[b_wg])
        for k in range(KC):
            S.dma("pool", lambda e: e.dma_start(out=wu[:, k, :], in_=wuv[:, k, :]), key=b_wu, writes=[b_wu])
        for k in range(4):
            S.dma("pool", lambda e: e.dma_start(out=wd[:, k, :], in_=wdv[:, k, :]), key=b_wd, writes=[b_wd])
        wts[ex] = (wg, b_wg, wu, b_wu, wd, b_wd)

    def emit_GU(ex, ci):
        t0, n = CHUNKS[ci]
        wg, b_wg, wu, b_wu, wd, b_wd = wts[ex]
        pgb, b_pgb = psG
        S.op("pe", lambda e: e.matmul(pgb[:, :n], lhsT=P.sel[:, ex, :], rhs=gT[:, t0:t0 + n], start=True, stop=True), reads=[P.b_const, b_gT], writes=[b_pgb])
        As = []
        for f in range(4):
            pG, b_pG = psA.next()
            pU, b_pU = psA.next()
            for k in range(KC):
                S.op("pe", lambda e: e.matmul(pG[:, :n], lhsT=wg[:, k, f * 128:(f + 1) * 128], rhs=hT[:, k, t0:t0 + n], start=(k == 0), stop=(k == KC - 1)),
                     reads=[b_wg, b_h[ci]], writes=[b_pG])
            for k in range(KC):
                S.op("pe", lambda e: e.matmul(pU[:, :n], lhsT=wu[:, k, f * 128:(f + 1) * 128], rhs=hT[:, k, t0:t0 + n], start=(k == 0), stop=(k == KC - 1)),
                     reads=[b_wu, b_h[ci]], writes=[b_pU])
            sl, b_sl = sil_ring.next()
            S.op("act", lambda e: e.activation(out=sl[:, :n], in_=pG[:, :n], func=AF.Silu), reads=[b_pG], writes=[b_sl])
            us, b_us = us_ring.next()
            S.op("dve", lambda e: e.tensor_tensor(out=us[:, :n], in0=pU[:, :n], in1=sl[:, :n], op=ALU.mult), reads=[b_pU, b_sl], writes=[b_us])
            aT, b_aT = a_ring.next()
            S.op("dve", lambda e: e.tensor_tensor(out=aT[:, :n], in0=us[:, :n], in1=pgb[:, :n], op=ALU.mult), reads=[b_us, b_pgb], writes=[b_aT])
            As.append((aT, b_aT))
        return As

    def emit_D(ex, ci, As):
        t0, n = CHUNKS[ci]
        t = 0 if ci == 0 else 1
        wg, b_wg, wu, b_wu, wd, b_wd = wts[ex]
        for m in range(KC):
            pD, b_pD = psD.next()
            for f in range(4):
                aT_f = As[f][0]
                S.op("pe", lambda e: e.matmul(pD[:, :n], lhsT=wd[:, f, m * 128:(m + 1) * 128], rhs=aT_f[:, :n], start=(f == 0), stop=(f == 3)),
                     reads=[b_wd, As[f][1]], writes=[b_pD])
            S.op("dve", lambda e: e.scalar_tensor_tensor(out=xT[:, m, t0:t0 + n], in0=pD[:, :n], scalar=modT[:, 40 + m, t:t + 1], in1=xT[:, m, t0:t0 + n],
                                                         op0=ALU.mult, op1=ALU.add), reads=[b_pD, b_mod, b_x[ci]], writes=[b_x[ci]])

    items = [(ex, ci) for ex in range(16) for ci in range(len(CHUNKS)) if not (skip0 and ci == 0)]
    load_expert(0)
    load_expert(1)
    cur = emit_GU(*items[0])
    for k, (ex, ci) in enumerate(items):
        nxt = None
        if k + 1 < len(items):
            ex2, ci2 = items[k + 1]
            nxt = emit_GU(ex2, ci2)
        emit_D(ex, ci, cur)
        if k + 1 < len(items) and items[k + 1][0] != ex and ex + 2 < 16:
            load_expert(ex + 2)
        cur = nxt

    if not last:
        for ci, (t0, n) in enumerate(CHUNKS):
            if skip0 and ci == 0:
                continue
            S.dma("sync", lambda e: e.dma_start(out=Xout[q][:, :, t0:t0 + n], in_=xT[:, :, t0:t0 + n]), key=b_x[ci], reads=[b_x[ci]])
    else:
        fg, b_fg = P.sb("fg", [128, KC], F32)
        S.dma("sync", lambda e: e.dma_start(out=fg[:], in_=D_["fgain"]), key=b_fg, writes=[b_fg])
        oring = tmp_ring
        for ci, (t0, n) in enumerate(CHUNKS):
            if ci == 0:
                continue
            ss, b_ss = psD.next()
            for k in range(KC):
                sq, b_sq = sq_ring.next()
                S.op("act", lambda e: e.activation(out=sq[:, :n], in_=xT[:, k, t0:t0 + n], func=AF.Square), reads=[b_x[ci]], writes=[b_sq])
                S.op("pe", lambda e: e.matmul(ss[:, :n], lhsT=P.ones[:], rhs=sq[:, :n], start=(k == 0), stop=(k == KC - 1)), reads=[b_sq, P.b_ones], writes=[b_ss])
            rs, b_rs = rs_ring.next()
            emit_rstd(P, ss, b_ss, n, 1.0 / D, rs, b_rs)
            for k in range(KC):
                fo, b_fo = oring.next()
                S.op("dve", lambda e: e.scalar_tensor_tensor(out=fo[:, :n], in0=xT[:, k, t0:t0 + n], scalar=fg[:, k:k + 1], in1=rs[:, :n],
                                                             op0=ALU.mult, op1=ALU.mult), reads=[b_x[ci], b_rs, b_fg], writes=[b_fo])
                S.dma("sync", lambda e: e.dma_start(out=P.out_d[q, :, k, t0 - CTX:t0 - CTX + n], in_=fo[:, :n]), key=b_fo, reads=[b_fo], is_out=True)
    P.release(mk)


NQ = 4


def build_fused(depth=DEPTH):
    P = Prog()
    S = P.S
    D_ = {}
    P.D = D_
    D_["xT"] = P.din("xT", [NQ, 128, KC, T], F32)
    D_["cvec"] = P.din("cvec", [128, KC, 2], F32)
    D_["w_ada"] = P.din("w_ada", [DEPTH, D, 6 * D], F32)
    D_["b_ada"] = P.din("b_ada", [DEPTH, 128, 48], F32)
    D_["w_in_even"] = P.din("w_in_even", [2, D, 1536], F32)
    D_["w_in_odd"] = P.din("w_in_odd", [2, D, 3072], F32)
    D_["w_out_even"] = P.din("w_out_even", [2, D, D], F32)
    D_["w_out_odd"] = P.din("w_out_odd", [2, D, D], F32)
    D_["gains"] = P.din("gains", [2, 128, 4], F32)
    D_["lamv"] = P.din("lamv", [2, 128, 4, 64], F32)
    D_["hgain"] = P.din("hgain", [2, 128, 1], F32)
    D_["wr"] = P.din("wr", [DEPTH, D, 20], F32)
    D_["br"] = P.din("br", [DEPTH, 128, 20], F32)
    D_["w_gate"] = P.din("w_gate", [DEPTH, 16, D, 512], F32)
    D_["w_up"] = P.din("w_up", [DEPTH, 16, D, 512], F32)
    D_["w_down"] = P.din("w_down", [DEPTH, 16, 512, D], F32)
    D_["fgain"] = P.din("fgain", [128, KC], F32)
    for nm in ("ropecos128", "ropesin128", "ropecos64", "ropesin64"):
        D_[nm] = P.din(nm, [NQ, 128, T], F32)
    cst_d = P.din("consts_bf", [128, 6, 128], BF16)
    sel_d = P.din("sel", [16, 16, 128], BF16)
    id_d = P.din("ident", [128, 128], F32)
    D_["cosL"] = P.din("cosL", [NQ, 4, 128, 64, 512], BF16)
    D_["nsinL"] = P.din("nsinL", [NQ, 4, 128, 64, 512], BF16)
    D_["cosC"] = P.din("cosC", [128, 2, 256], BF16)
    D_["nsinC"] = P.din("nsinC", [128, 2, 256], BF16)
    P.out_d = P.dout("out", [NQ, 128, KC, TL], F32)
    X1 = P.dscr("X1", [NQ, 128, KC, T], F32)
    X2 = P.dscr("X2", [NQ, 128, KC, T], F32)
    X3 = P.dscr("X3", [NQ, 128, KC, T], F32)
    P.Qs = P.dscr("Qs", [NQ, 8, 128, T], BF16)
    P.Ks = P.dscr("Ks", [8, 128, NKEY], BF16)
    P.Vs = P.dscr("Vs", [128, NKT, 256], BF16)
    P.Vo = P.dscr("Vo", [8, 128, NKT, 128], BF16)
    P.GCs = P.dscr("GCs", [128, 64, 256], BF16)
    P.GSs = P.dscr("GSs", [128, 64, 256], BF16)
    P.GCc = P.dscr("GCc", [128, 2, 256], BF16)
    P.GSc = P.dscr("GSc", [128, 2, 256], BF16)

    P.bank = [P.ps("bank%d" % b) for b in range(8)]
    P.ones, P.b_ones = P.sb("ones", [128, 128], BF16)
    S.op("pool", lambda e: e.memset(P.ones[:], 1.0), writes=[P.b_ones])
    P.ones32, _ = P.sb("ones32", [128, 128], F32)
    S.op("pool", lambda e: e.memset(P.ones32[:], 1.0), writes=[P.b_ones])
    P.epsc, P.b_eps = P.sb("epsc", [128, 1], F32)
    S.op("pool", lambda e: e.memset(P.epsc[:], EPS), writes=[P.b_eps])
    P.b_const = Buf("const")
    P.biasm8, _ = P.sb("biasm8", [128, 1], F32)
    S.op("pool", lambda e: e.memset(P.biasm8[:], -8.0), writes=[P.b_const])
    P.bias0, _ = P.sb("bias0", [128, 1], F32)
    S.op("pool", lambda e: e.memset(P.bias0[:], 0.0), writes=[P.b_const])
    cst, _ = P.sb("cst", [128, 6, 128], BF16)
    S.dma("sync", lambda e: e.dma_start(out=cst[:], in_=cst_d), key=P.b_const, writes=[P.b_const])
    P.PT128, P.PT64, P.bdc, P.bds, P.bdo = cst[:, 0, :], cst[:, 1, :], cst[:, 2, :], cst[:, 3, :], cst[:, 4, :]
    P.sel, _ = P.sb("sel", [16, 16, 128], BF16)
    S.dma("sync", lambda e: e.dma_start(out=P.sel[:], in_=sel_d), key=P.b_const, writes=[P.b_const])
    P.ident, _ = P.sb("ident", [128, 128], F32)
    S.dma("sync", lambda e: e.dma_start(out=P.ident[:], in_=id_d), key=P.b_const, writes=[P.b_const])
    P.modT, P.b_mod = P.sb("modT", [128, 48, 2], F32)
    P.scp, _ = P.sb("scp", [128, 48, 2], F32)
    P.nlam, P.b_lam = P.sb("nlam", [128, 1], F32)
    P.hg, _ = P.sb("hg", [128, 1], F32)
    cv, b_cv = P.sb("cv", [128, KC, 2], F32)
    S.dma("sync", lambda e: e.dma_start(out=cv[:], in_=D_["cvec"]), key=b_cv, writes=[b_cv])
    P.cvb, P.b_cvb = P.sb("cvb", [128, KC, 2], BF16)
    S.op("act", lambda e: e.activation(out=P.cvb[:], in_=cv[:], func=AF.Silu), reads=[b_cv], writes=[P.b_cvb])
    S.fence()

    Xin = D_["xT"]
    for l in range(depth):
        last = (l == DEPTH - 1)
        even = (l % 2 == 0)
        Xmid = X1
        Xout = X2 if even else X3
        phase_mod(P, l)
        if not even:
            phase_lam(P, l)
        for q in range(NQ):
            phase_A(P, l, q, Xin)
        for q in range(NQ):
            if even:
                phase_B_even(P, l, q, Xin, Xmid)
            else:
                phase_B_odd(P, l, q, Xin, Xmid)
        for q in range(NQ):
            phase_C(P, l, q, Xmid, Xout, last)
        Xin = Xout
    if depth < DEPTH:
        xdbg = P.dout("xdbg", [NQ, 128, KC, T], F32)
        bdbg = Buf("dbg")
        for q in range(NQ):
            S.dma("sync", lambda e: e.dma_start(out=xdbg[q], in_=Xin[q]), key=bdbg, writes=[bdbg], is_out=True)
    return P.finish()


def _to_fm(xtok):
    return np.ascontiguousarray(xtok.reshape(xtok.shape[0], KC, 128).transpose(2, 1, 0))


def _from_fm(xfm):
    return np.ascontiguousarray(xfm.transpose(2, 1, 0).reshape(xfm.shape[2], KC * 128))


def _rope_tables(dim, core_q):
    half = dim // 2
    nf = half // 2
    inv = (10000.0 ** (-np.arange(0, half, 2, dtype=np.float32) / np.float32(half))).astype(np.float32)
    tok = np.arange(core_q * TL, (core_q + 1) * TL)
    row = (tok // 64).astype(np.float32)
    col = (tok % 64).astype(np.float32)
    ang_r = row[:, None] * inv[None, :]
    ang_c = col[:, None] * inv[None, :]
    cosT = np.ones((128, T), np.float32)
    sinT = np.zeros((128, T), np.float32)
    d = np.arange(128)
    u = d % dim
    is_col = u >= half
    fidx = u % nf
    cr, sr, cc, sc = np.cos(ang_r), np.sin(ang_r), np.cos(ang_c), np.sin(ang_c)
    cosT[:, CTX:] = np.where(is_col[:, None], cc[:, fidx].T, cr[:, fidx].T)
    sinT[:, CTX:] = np.where(is_col[:, None], sc[:, fidx].T, sr[:, fidx].T)
    PT = np.zeros((128, 128), np.float32)
    for m in range(128):
        if (m % half) < nf:
            PT[m + nf, m] = -1.0
        else:
            PT[m - nf, m] = 1.0
    return cosT, sinT, PT.astype(NPBF)


_CACHE = {}


def _dft_tables():
    if "dft" in _CACHE:
        return _CACHE["dft"]
    n = np.arange(SEQ, dtype=np.int64)
    scl = 1.0 / math.sqrt(SEQ * 64)
    cosL = np.empty((NQ, 4, 128, 64, 512), NPBF)
    nsinL = np.empty((NQ, 4, 128, 64, 512), NPBF)
    for q in range(NQ):
        k = np.arange(q * TL, (q + 1) * TL, dtype=np.int64)
        ang = (2.0 * np.pi / SEQ) * ((n[:, None] * k[None, :]) % SEQ).astype(np.float64)
        c = (np.cos(ang) * scl).astype(np.float32).astype(NPBF)
        s = (-np.sin(ang) * scl).astype(np.float32).astype(NPBF)
        cosL[q] = c.reshape(64, 128, 4, 512).transpose(2, 1, 0, 3)
        nsinL[q] = s.reshape(64, 128, 4, 512).transpose(2, 1, 0, 3)
    nn = np.arange(CTX, dtype=np.int64)
    ang = (2.0 * np.pi / CTX) * ((nn[:, None] * nn[None, :]) % CTX).astype(np.float64)
    sc2 = 1.0 / math.sqrt(CTX * 64)
    cC = np.ascontiguousarray((np.cos(ang) * sc2).astype(np.float32).astype(NPBF).reshape(2, 128, CTX).transpose(1, 0, 2))
    sC = np.ascontiguousarray((-np.sin(ang) * sc2).astype(np.float32).astype(NPBF).reshape(2, 128, CTX).transpose(1, 0, 2))
    cc = np.arange(64)
    a64 = (2.0 * np.pi / 64) * ((cc[:, None] * cc[None, :]) % 64)
    bdc = np.zeros((128, 128), np.float32)
    bds = np.zeros((128, 128), np.float32)
    bdo = np.zeros((128, 128), np.float32)
    for g in range(2):
        bdc[g * 64:(g + 1) * 64, g * 64:(g + 1) * 64] = np.cos(a64)
        bds[g * 64:(g + 1) * 64, g * 64:(g + 1) * 64] = np.sin(a64)
        bdo[g * 64:(g + 1) * 64, g * 64:(g + 1) * 64] = 1.0
    _CACHE["dft"] = (cosL, nsinL, cC, sC, bdc.astype(NPBF), bds.astype(NPBF), bdo.astype(NPBF))
    return _CACHE["dft"]


def _host_inputs(x, c, ctx, c_ctx, w_ada, b_ada, w_in_even, w_out_even, a_q_gain, a_k_gain, b_gain,
                 w_in_odd, w_out_odd, c_lambda_q1, c_lambda_k1, c_lambda_q2, c_lambda_k2, c_head_gain,
                 w_group, b_group, w_router, b_router, w_gate, w_up, w_down, final_gain):
    f32 = np.float32
    A = lambda a: np.ascontiguousarray(np.asarray(a, f32))
    x, ctx, c, c_ctx = A(x), A(ctx), A(c), A(c_ctx)
    cosL, nsinL, cC, sC, bdc, bds, bdo = _dft_tables()
    r128 = [_rope_tables(128, q) for q in range(NQ)]
    r64 = [_rope_tables(64, q) for q in range(NQ)]
    consts_bf = np.ascontiguousarray(np.stack([r128[0][2], r64[0][2], bdc, bds, bdo, np.eye(128, dtype=f32).astype(NPBF)], axis=1))
    sel = np.zeros((16, 16, 128), f32)
    for e in range(16):
        sel[e, e, :] = 1.0
    shared = {
        "w_ada": A(w_ada), "b_ada": np.ascontiguousarray(A(b_ada).reshape(DEPTH, 48, 128).transpose(0, 2, 1)),
        "w_in_even": A(w_in_even), "w_in_odd": A(w_in_odd), "w_out_even": A(w_out_even), "w_out_odd": A(w_out_odd),
        "gains": np.ascontiguousarray(np.stack([np.concatenate([A(a_q_gain)[i][:, None], A(a_k_gain)[i][:, None], A(b_gain)[i].reshape(2, 128).T], axis=1)
                                                for i in range(2)], axis=0)),
        "lamv": np.ascontiguousarray(np.stack([np.broadcast_to(np.stack([A(a_)[i] for a_ in (c_lambda_q1, c_lambda_k1, c_lambda_q2, c_lambda_k2)], 0)[None], (128, 4, 64))
                                               for i in range(2)], axis=0)),
        "hgain": np.ascontiguousarray(A(c_head_gain)[:, :, None]),
        "wr": np.ascontiguousarray(np.concatenate([A(w_group), A(w_router)], axis=2)),
        "br": np.ascontiguousarray(np.broadcast_to(np.concatenate([A(b_group), A(b_router)], axis=1)[:, None, :], (DEPTH, 128, 20))),
        "w_gate": A(w_gate), "w_up": A(w_up), "w_down": A(w_down),
        "fgain": np.ascontiguousarray(A(final_gain).reshape(KC, 128).T),
        "ropecos128": np.ascontiguousarray(np.stack([r[0] for r in r128])), "ropesin128": np.ascontiguousarray(np.stack([r[1] for r in r128])),
        "ropecos64": np.ascontiguousarray(np.stack([r[0] for r in r64])), "ropesin64": np.ascontiguousarray(np.stack([r[1] for r in r64])),
        "consts_bf": consts_bf, "sel": sel.astype(NPBF), "ident": np.eye(128, dtype=f32),
        "cosL": cosL, "nsinL": nsinL, "cosC": cC, "nsinC": sC,
    }
    per_batch = []
    for b in range(2):
        xq = np.stack([_to_fm(np.concatenate([ctx[b], x[b, q * TL:(q + 1) * TL]], axis=0)) for q in range(NQ)], axis=0)
        cvec = np.ascontiguousarray(np.stack([c_ctx, c[b]], axis=-1).reshape(KC, 128, 2).transpose(1, 0, 2))
        m = dict(shared)
        m["xT"] = xq
        m["cvec"] = cvec
        per_batch.append(m)
    return [per_batch[core // 4] for core in range(NCORES)]


def kernel(**inputs):
    if "nc" not in _CACHE:
        _CACHE["nc"] = build_fused()
    maps = _host_inputs(**inputs)
    res = run_bass_kernel_spmd(_CACHE["nc"], maps, core_ids=list(range(NCORES))).results
    out = np.zeros((2, SEQ, D), np.float32)
    for b in range(2):
        o = res[4 * b]["out"]
        for q in range(NQ):
            out[b, q * TL:(q + 1) * TL] = _from_fm(o[q])
    return out
```
